# Optimizing a Trainium2 kernel written in Bass

```python
import math
import jax, jax.numpy as jnp
from jax import lax
import numpy as np

D_MODEL = 1024
BATCH = 8
SEQ = 2048
DEPTH = 4

N_MIXERS = 2
N_MEM = 256
XA_HEADS = 4
XA_WIDTH = D_MODEL // 4
XA_HEAD_DIM = XA_WIDTH // XA_HEADS
MIX_WIDTH = D_MODEL - XA_WIDTH
GLA_HEADS = 4
GLA_DV = MIX_WIDTH
GLA_DK = MIX_WIDTH // 2
GLA_HEAD_K = GLA_DK // GLA_HEADS
GLA_HEAD_V = GLA_DV // GLA_HEADS
GLA_GATE_RANK = 16
GLA_GATE_TAU = 16.0
GLA_CHUNK = 64
CONV_GROUPS = 4
CONV_WIDTH = 3
D_FF = int(math.ceil(8 * D_MODEL / 3 / 128)) * 128
FFN_CONV_WIDTH = 3
LN_EPS = 1e-5
DEEPNORM_ALPHA = (2 * DEPTH) ** 0.25
DEEPNORM_BETA = (8 * DEPTH) ** -0.25
N_GLA_LAYERS = (DEPTH + 1) // 2
N_CONV_LAYERS = DEPTH // 2
GLA_IN_SIZES = (GLA_DK, GLA_DK, GLA_DV, GLA_DV, GLA_GATE_RANK, XA_WIDTH)
CONV_IN_SIZES = (MIX_WIDTH, MIX_WIDTH, MIX_WIDTH, XA_WIDTH)
GLA_IN = sum(GLA_IN_SIZES)
CONV_IN = sum(CONV_IN_SIZES)

kernel_name = "hybrid_gla_shortconv_memxattn"


def split_cols(t, sizes):
    idx = np.cumsum(np.array(sizes))[:-1].tolist()
    return jnp.split(t, idx, axis=-1)


def layer_norm(x, g, b):
    xf = x.astype(jnp.float32)
    mu = jnp.mean(xf, axis=-1, keepdims=True)
    var = jnp.mean(jnp.square(xf - mu), axis=-1, keepdims=True)
    return ((xf - mu) * lax.rsqrt(var + LN_EPS)).astype(x.dtype) * g + b


def causal_dwconv(x, w):
    K = w.shape[0]
    S = x.shape[1]
    xp = jnp.pad(x, ((0, 0), (K - 1, 0), (0, 0)))
    y = xp[:, 0:S, :] * w[0]
    for kk in range(1, K):
        y = y + xp[:, kk:kk + S, :] * w[kk]
    return y


def gla_mixer(q, k, v, r, gate_lr, w_a2, b_a, head_g):
    bsz, seq, _ = q.shape
    n_chunks = seq // GLA_CHUNK
    f32 = jnp.float32

    def to_chunks(t, dh):
        return t.reshape(bsz, n_chunks, GLA_CHUNK, GLA_HEADS, dh).transpose(0, 3, 1, 2, 4)

    log_a = jax.nn.log_sigmoid((gate_lr @ w_a2 + b_a).astype(f32)) / GLA_GATE_TAU
    G = jnp.cumsum(to_chunks(log_a, GLA_HEAD_K), axis=3)
    G_last = G[:, :, :, -1:, :]
    qc = to_chunks(q, GLA_HEAD_K).astype(f32) * (GLA_HEAD_K ** -0.5)
    kc = to_chunks(k, GLA_HEAD_K).astype(f32)
    vc = to_chunks(v, GLA_HEAD_V).astype(f32)

    q_dec = qc * jnp.exp(G)
    k_inv = kc * jnp.exp(-G)
    k_end = kc * jnp.exp(G_last - G)

    causal = jnp.tril(jnp.ones((GLA_CHUNK, GLA_CHUNK), dtype=bool))
    att = jnp.einsum('bhnik,bhnjk->bhnij', q_dec, k_inv)
    att = jnp.where(causal, att, 0.0)
    o_intra = jnp.einsum('bhnij,bhnjv->bhniv', att, vc)

    kv_chunk = jnp.einsum('bhnck,bhncv->bhnkv', k_end, vc)
    decay_chunk = jnp.exp(G_last[:, :, :, 0, :])

    def step(state, inp):
        kv_n, d_n = inp
        return d_n[..., None] * state + kv_n, state

    init = jnp.zeros((bsz, GLA_HEADS, GLA_HEAD_K, GLA_HEAD_V), f32)
    _, states = lax.scan(step, init, (jnp.moveaxis(kv_chunk, 2, 0), jnp.moveaxis(decay_chunk, 2, 0)))
    states = jnp.moveaxis(states, 0, 2)
    o_inter = jnp.einsum('bhnck,bhnkv->bhncv', q_dec, states)

    o = o_intra + o_inter
    o = o * lax.rsqrt(jnp.mean(jnp.square(o), axis=-1, keepdims=True) + LN_EPS)
    o = o.transpose(0, 2, 3, 1, 4).reshape(bsz, seq, GLA_DV).astype(v.dtype) * head_g
    return o * jax.nn.silu(r)


def short_conv_mixer(b_gate, c_gate, h, conv_w):
    return b_gate * causal_dwconv(c_gate * h, conv_w)


def memory_xattn(q, mem_kv):
    bsz, seq, _ = q.shape
    m = mem_kv.shape[1]
    k, v = jnp.split(mem_kv, 2, axis=-1)
    qh = q.reshape(bsz, seq, XA_HEADS, XA_HEAD_DIM)
    kh = k.reshape(bsz, m, XA_HEADS, XA_HEAD_DIM)
    vh = v.reshape(bsz, m, XA_HEADS, XA_HEAD_DIM)
    s = jnp.einsum('bshd,bmhd->bhsm', qh, kh).astype(jnp.float32) * (XA_HEAD_DIM ** -0.5)
    p = jax.nn.softmax(s, axis=-1).astype(v.dtype)
    o = jnp.einsum('bhsm,bmhd->bshd', p, vh)
    return o.reshape(bsz, seq, XA_WIDTH)


def conv_ffn(x, w_up, conv_w, conv_b, w_down):
    u = causal_dwconv(x @ w_up, conv_w) + conv_b
    gate, val = jnp.split(u, 2, axis=-1)
    return (jax.nn.silu(gate) * val) @ w_down


def setup_inputs(seed: int = 0) -> dict:
    key = jax.random.key(seed)
    ks = jax.random.split(key, 24)

    def nrm(k, shape, scale):
        return jax.random.normal(k, shape, jnp.float32) * scale

    d_inv = D_MODEL ** -0.5
    return {
        "x": nrm(ks[0], (BATCH, SEQ, D_MODEL), 1.0),
        "mem": nrm(ks[1], (BATCH, N_MEM, D_MODEL), 1.0),
        "gla_w_in": nrm(ks[2], (N_GLA_LAYERS, D_MODEL, GLA_IN), d_inv),
        "gla_w_a2": nrm(ks[3], (N_GLA_LAYERS, GLA_GATE_RANK, GLA_DK), GLA_GATE_RANK ** -0.5),
        "gla_b_a": nrm(ks[4], (N_GLA_LAYERS, GLA_DK), 0.01),
        "gla_head_g": 1.0 + nrm(ks[5], (N_GLA_LAYERS, GLA_DV), 0.02),
        "gla_w_out": nrm(ks[6], (N_GLA_LAYERS, D_MODEL, D_MODEL), d_inv * DEEPNORM_BETA),
        "conv_w_in": nrm(ks[7], (N_CONV_LAYERS, D_MODEL, CONV_IN), d_inv),
        "conv_w": nrm(ks[8], (N_CONV_LAYERS, CONV_WIDTH, MIX_WIDTH), CONV_WIDTH ** -0.5),
        "conv_w_out": nrm(ks[9], (N_CONV_LAYERS, D_MODEL, D_MODEL), d_inv * DEEPNORM_BETA),
        "w_mem_kv": nrm(ks[10], (DEPTH, D_MODEL, 2 * XA_WIDTH), d_inv),
        "ln1_g": 1.0 + nrm(ks[11], (DEPTH, D_MODEL), 0.02),
        "ln1_b": nrm(ks[12], (DEPTH, D_MODEL), 0.02),
        "ffn_w_up": nrm(ks[13], (DEPTH, D_MODEL, 2 * D_FF), d_inv),
        "ffn_conv_w": nrm(ks[14], (DEPTH, FFN_CONV_WIDTH, 2 * D_FF), FFN_CONV_WIDTH ** -0.5),
        "ffn_conv_b": nrm(ks[15], (DEPTH, 2 * D_FF), 0.01),
        "ffn_w_down": nrm(ks[16], (DEPTH, D_FF, D_MODEL), (D_FF ** -0.5) * DEEPNORM_BETA),
        "ln2_g": 1.0 + nrm(ks[17], (DEPTH, D_MODEL), 0.02),
        "ln2_b": nrm(ks[18], (DEPTH, D_MODEL), 0.02),
    }


def reference(x, mem, gla_w_in, gla_w_a2, gla_b_a, gla_head_g, gla_w_out,
              conv_w_in, conv_w, conv_w_out, w_mem_kv, ln1_g, ln1_b,
              ffn_w_up, ffn_conv_w, ffn_conv_b, ffn_w_down, ln2_g, ln2_b):
    for i in range(DEPTH):
        j = i // N_MIXERS
        mem_kv = mem @ w_mem_kv[i]
        if i % N_MIXERS == 0:
            proj = x @ gla_w_in[j]
            q, k, v, r, gate_lr, mem_q = split_cols(proj, GLA_IN_SIZES)
            mix = gla_mixer(q, k, v, r, gate_lr, gla_w_a2[j], gla_b_a[j], gla_head_g[j])
            w_out = gla_w_out[j]
        else:
            proj = x @ conv_w_in[j]
            b_gate, c_gate, h, mem_q = split_cols(proj, CONV_IN_SIZES)
            mix = short_conv_mixer(b_gate, c_gate, h, conv_w[j])
            w_out = conv_w_out[j]
        xa = memory_xattn(mem_q, mem_kv)
        y = jnp.concatenate([mix, xa], axis=-1) @ w_out
        x = layer_norm(DEEPNORM_ALPHA * x + y, ln1_g[i], ln1_b[i])
        f = conv_ffn(x, ffn_w_up[i], ffn_conv_w[i], ffn_conv_b[i], ffn_w_down[i])
        x = layer_norm(DEEPNORM_ALPHA * x + f, ln2_g[i], ln2_b[i])
    return x
```

```python
import numpy as np
from contextlib import ExitStack
import concourse.bass as bass
import concourse.mybir as mybir
from concourse.bass_utils import run_bass_kernel_spmd

F32 = mybir.dt.float32
BF16 = mybir.dt.bfloat16
AF = mybir.ActivationFunctionType
ALU = mybir.AluOpType

T = 2048
D = 1024
KC = 8
NMEM = 256
TTM = 256
TTF = 512
NPAR = 240
ALPHA = float(8.0 ** 0.25)
EPS = 1e-5
QSCALE = float(96.0 ** -0.5)
GROUPS = [(0, 9), (9, 9), (18, 4)]
USZ = 20608
NDMA = 24
NIN = {0: 2576, 1: 2560}
NKO = {0: 10, 1: 8}


class Trk:
    def __init__(self, nc, es):
        self.nc = nc
        self.engs = {"pe": nc.tensor, "act": nc.scalar, "dve": nc.vector,
                     "pool": nc.gpsimd, "sp": nc.sync}
        self.sems = {k: es.enter_context(nc.semaphore("c_" + k)) for k in self.engs}
        self.cnt = {k: 0 for k in self.engs}
        self.seen = {k: {} for k in self.engs}
        self.lastw = {}
        self.readers = {}
        self.dsem = [es.enter_context(nc.semaphore("d%d" % i)) for i in range(NDMA)]
        self.dval = [0] * NDMA
        self.dnext = 0
        self.nops = 0

    def _semh(self, key):
        if isinstance(key, tuple):
            return self.dsem[key[1]]
        return self.sems[key]

    def _wait(self, eng, tok):
        key, val = tok
        if key == "pe" and eng == "pe":
            return
        if self.seen[eng].get(key, 0) >= val:
            return
        self.seen[eng][key] = val
        self.engs[eng].wait_ge(self._semh(key), val)

    def _deps(self, eng, reads, writes):
        for r in reads:
            if r in self.lastw:
                self._wait(eng, self.lastw[r])
        for w in writes:
            if w in self.lastw:
                self._wait(eng, self.lastw[w])
            for k, v in self.readers.get(w, {}).items():
                self._wait(eng, (k, v))

    def _commit(self, tok, reads, writes):
        for r in reads:
            d = self.readers.setdefault(r, {})
            if d.get(tok[0], 0) < tok[1]:
                d[tok[0]] = tok[1]
        for w in writes:
            self.lastw[w] = tok
            self.readers[w] = {}

    def op(self, eng, fn, reads=(), writes=(), sig=True):
        self._deps(eng, reads, writes)
        ins = fn(self.engs[eng])
        self.nops += 1
        if sig:
            self.cnt[eng] += 1
            tok = (eng, self.cnt[eng])
            ins.then_inc(self.sems[eng], 1)
        else:
            tok = (eng, self.cnt[eng] + 1)
        self._commit(tok, reads, writes)
        return tok

    def dma(self, q, out, in_, reads=(), writes=()):
        self._deps(q, reads, writes)
        i = self.dnext
        self.dnext = (i + 1) % NDMA
        if self.dval[i] > 0:
            self._wait(q, (("d", i), self.dval[i]))
        ins = self.engs[q].dma_start(out=out, in_=in_)
        self.dval[i] += 16
        tok = (("d", i), self.dval[i])
        ins.then_inc(self.dsem[i], 16)
        self.nops += 1
        self._commit(tok, reads, writes)
        return tok

    def wait_all(self, eng, res):
        for r in res:
            if r in self.lastw:
                self._wait(eng, self.lastw[r])


class Builder:
    def __init__(self, layers):
        self.layers = list(layers)
        nc = self.nc = bass.Bass("TRN2", target_bir_lowering=False)
        self.es = ExitStack()
        es = self.es

        def dt(name, shape, kind="ExternalInput"):
            return nc.dram_tensor(name, shape, F32, kind=kind).ap()

        self.xT = dt("xT", [D, T])
        self.memT = dt("memT", [D, NMEM])
        self.consts_d = dt("consts", [128, 384])
        self.params_d = dt("params", [128, 4 * NPAR])
        self.win_d = {0: dt("win_g", [2, 128, KC * 2576]), 1: dt("win_c", [2, 128, KC * 2560])}
        self.wout_d = {0: dt("wout_g", [2, 128, 10 * 1024]), 1: dt("wout_c", [2, 128, 8 * 1024])}
        self.wa2_d = dt("wa2", [2, 17, 384])
        self.wkv_d = dt("wkv", [4, 128, KC * 512])
        self.wup_d = dt("wup", [4, 22, 128, KC * 256])
        self.wdn_d = dt("wdn", [4, 8, 128, 22 * 128])
        self.yT = dt("yT", [D, T], kind="ExternalOutput")

        def sb(name, shape, dtype):
            return es.enter_context(nc.sbuf_tensor(name, shape, dtype))

        self.x32 = sb("x32", [128, KC * T], F32)
        self.U = sb("U", [128, USZ], BF16)
        self.W2 = sb("W2", [128, 10240], BF16)
        self.MIb = sb("MIb", [128, KC * T], BF16)
        self.MIf = sb("MIf", [128, 4224], F32)
        self.consts = sb("consts_s", [128, 384], F32)
        self.params = sb("params_s", [128, 4 * NPAR], F32)
        self.ones = sb("ones", [128, 128], BF16)
        self.memTb = sb("memTb", [128, KC * NMEM], BF16)
        self.memK = sb("memK", [128, 2 * NMEM], BF16)
        self.memV = sb("memV", [128, 2 * 256], BF16)
        self.wa2 = sb("wa2s", [17, 384], F32)
        self.halo = sb("halo", [128, 16], F32)
        self.scr = sb("scr", [128, 8], F32)
        self.rcb = sb("rcb", [128, 512], F32)
        self.tmpA = sb("tmpA", [128, 512], F32)
        self.tmpB = sb("tmpB", [128, 512], F32)
        self.tmpC = sb("tmpC", [128, 512], F32)
        self.tmpD = sb("tmpD", [128, 512], F32)
        self.negmean = sb("negmean", [128, TTM], F32)
        self.rstd = sb("rstd", [128, TTM], F32)
        self.nmr = sb("nmr", [128, TTM], F32)
        self.lnz = sb("lnz", [128, 2 * KC * TTM], BF16)
        self.bg = []
        self.ps = [es.enter_context(nc.psum_tensor("ps%d" % i, [128, 512], F32)) for i in range(8)]

        self.t = Trk(nc, es)
        self.bank_ctr = {}

        tt = TTM
        o = 0

        def carve(n):
            nonlocal o
            a = o
            o += n
            return a
        a0 = carve(KC * tt)
        self.o_xbt = [a0, a0]
        self.o_qd = carve(4 * tt)
        self.o_ki = carve(4 * tt)
        self.o_kend = carve((tt // 128) * 384)
        self.o_vtok = carve((tt // 128) * 768)
        self.o_att = carve(4 * 128)
        self.o_stb = carve(4 * 2 * 192)
        self.o_memq = carve(2 * tt)
        self.o_expp = carve(2 * 2 * tt)
        self.o_mix = carve(10 * tt)
        self.o_sq = carve(2 * tt)
        self.o_sq2 = carve(2 * tt)
        self.o_wkv = self.o_qd
        assert self.o_qd + 4096 <= self.o_stb
        assert o <= KC * T, o
        o = 0
        self.f_xg = carve(tt)
        self.f_ltok = carve(384)
        self.f_et = carve(tt)
        self.f_eend = carve(384)
        self.f_state = carve(4 * 192)
        self.f_decay = carve(16)
        self.f_rrms = carve(tt)
        self.f_rrms2 = carve(tt)
        self.f_hs = [carve(tt), carve(tt)]
        self.f_ch = [carve(tt + 2), carve(tt + 2)]
        assert o <= 4224, o
        self.lay = {0: {k: getattr(self, k) for k in ("o_xbt", "o_memq", "o_expp", "o_mix", "o_wkv", "f_hs", "f_ch")}}
        c512 = 512
        self.lay[1] = {"o_xbt": [0, 0], "o_memq": KC * c512, "o_expp": KC * c512 + 2 * c512,
                       "o_mix": KC * c512 + 2 * c512 + 4 * c512, "o_wkv": KC * c512 + 2 * c512 + 4 * c512 + 8 * c512,
                       "f_hs": [0, c512], "f_ch": [2 * c512, 3 * c512 + 2]}
        assert self.lay[1]["o_wkv"] + 4096 <= KC * T and 4 * c512 + 4 <= 4224
        self.f_ug = 0
        self.f_uv = 2 + T + 62
        assert self.f_uv + 2 + T <= 4224

    def bank(self, lo=0, hi=8):
        k = (lo, hi)
        c = self.bank_ctr.get(k, 0)
        self.bank_ctr[k] = c + 1
        return lo + (c % (hi - lo))

    def mm_group(self, out_ap, out_res, pairs, reads):
        n = len(pairs)
        for i, (l, r) in enumerate(pairs):
            self.t.op("pe", lambda e, l=l, r=r, i=i: e.matmul(out_ap, l, r, start=(i == 0), stop=(i == n - 1)),
                      reads=reads, writes=[out_res], sig=(i == n - 1))

    def par(self, l, col, rows=128):
        c = l * NPAR + col
        return self.params[0:rows, c:c + 1]

    def barrier(self, toks):
        self.t.op("dve", lambda e: e.memset(self.scr[:, 0:1], 0.0), reads=[], writes=list(toks) + ["scr"])

    def prologue(self):
        t = self.t
        t.dma("sp", self.consts[:], self.consts_d, writes=["consts"])
        t.dma("sp", self.params[:], self.params_d, writes=["params"])
        xv = self.xT.rearrange("(kc p) t -> p kc t", p=128)
        x3 = self.x32[:].rearrange("p (kc t) -> p kc t", kc=KC)
        t.dma("sp", x3[:, :, 0:TTM], xv[:, :, 0:TTM], writes=[("x32", 0)])
        self.x_rest = [(x3[:, :, q * TTM:(q + 1) * TTM], xv[:, :, q * TTM:(q + 1) * TTM], q) for q in range(1, T // TTM)]
        mv = self.memT.rearrange("(kc p) m -> p kc m", p=128)
        t.dma("pool", self.memTb[:].rearrange("p (kc m) -> p kc m", kc=KC), mv, writes=["memTb"])
        t.op("dve", lambda e: e.memset(self.ones[:], 1.0), writes=["ones"])
        t.op("dve", lambda e: e.memset(self.scr[:], 0.0), writes=["scr", "scr1"])

    def ublk(self, lo, hi):
        return [("U", q) for q in range(lo // 1024, (hi - 1) // 1024 + 1)]

    def load_win(self, l, pieces):
        t = self.t
        typ = l % 2
        j = l // 2
        nin = NIN[typ]
        step = KC * nin // 4
        for pc in pieces:
            t.dma("pool", self.U[:, pc * step:(pc + 1) * step], self.win_d[typ][j, :, pc * step:(pc + 1) * step],
                  reads=[], writes=self.ublk(pc * step, (pc + 1) * step))

    def load_wout(self, l):
        t = self.t
        typ = l % 2
        j = l // 2
        nko = NKO[typ]
        half = nko * 1024 // 2
        for pc in range(2):
            t.dma("pool", self.W2[:, pc * half:(pc + 1) * half], self.wout_d[typ][j, :, pc * half:(pc + 1) * half],
                  reads=[], writes=(["W2_own"] if pc == 0 else []) + [("wout", pc)])
        if typ == 0:
            t.dma("sp", self.wa2[:], self.wa2_d[j], writes=["wa2"])

    def win(self, typ, kc, c0, n):
        nin = NIN[typ]
        a = kc * nin + c0
        return self.U[:, a:a + n]

    def win_res(self, typ, c0=None, n=None):
        nin = NIN[typ]
        r = []
        for kc in range(KC):
            for x in self.ublk(kc * nin + c0, kc * nin + c0 + n):
                if x not in r:
                    r.append(x)
        return r

    def wout(self, typ, i, o, rows):
        a = i * 1024 + o * 128
        return self.W2[0:rows, a:a + 128]

    def mem_kv(self, l):
        t = self.t
        wk = self.MIb[:, self.o_wkv:self.o_wkv + 4096]
        t.dma("pool", wk, self.wkv_d[l], reads=["MI_own"], writes=["wkv"])
        for hp in range(2):
            b = self.bank()
            pairs = [(self.MIb[:, self.o_wkv + kc * 512 + hp * 128: self.o_wkv + kc * 512 + hp * 128 + 128],
                      self.memTb[:, kc * NMEM:(kc + 1) * NMEM]) for kc in range(KC)]
            self.mm_group(self.ps[b][:, 0:NMEM], ("ps", b), pairs, ["wkv", "memTb", "MI_own"])
            t.op("act", lambda e, b=b, hp=hp: e.copy(self.memK[:, hp * NMEM:(hp + 1) * NMEM], self.ps[b][:, 0:NMEM]),
                 reads=[("ps", b)], writes=["memK"])
        for mc in range(2):
            b = self.bank()
            pairs = [(self.memTb[:, kc * NMEM + mc * 128: kc * NMEM + mc * 128 + 128],
                      self.MIb[:, self.o_wkv + kc * 512 + 256: self.o_wkv + kc * 512 + 512]) for kc in range(KC)]
            self.mm_group(self.ps[b][:, 0:256], ("ps", b), pairs, ["wkv", "memTb", "MI_own"])
            t.op("act", lambda e, b=b, mc=mc: e.copy(self.memV[:, mc * 256:(mc + 1) * 256], self.ps[b][:, 0:256]),
                 reads=[("ps", b)], writes=["memV"])

    def inproj_fm(self, typ, c0, m, tt, out_ap, out_res):
        xo = self.o_xbt[self.xb]
        pairs = [(self.win(typ, kc, c0, m), self.MIb[:, xo + kc * tt: xo + (kc + 1) * tt])
                 for kc in range(KC)]
        self.mm_group(out_ap, out_res, pairs, self.win_res(typ, c0, m) + ["xbt", "MI_own"])
        self.tick()

    def inproj_tm(self, typ, c0, n, tt, c, out_ap, out_res):
        xo = self.o_xbt[self.xb]
        pairs = [(self.MIb[:, xo + kc * tt + c * 128: xo + kc * tt + (c + 1) * 128],
                  self.win(typ, kc, c0, n)) for kc in range(KC)]
        self.mm_group(out_ap, out_res, pairs, self.win_res(typ, c0, n) + ["xbt", "MI_own"])
        self.tick()

    def cast_tile(self, ti, tt):
        t = self.t
        t0 = ti * tt
        self.xb = ti % 2
        xo = self.o_xbt[self.xb]
        for kc in range(KC):
            src = self.x32[:, kc * T + t0: kc * T + t0 + tt]
            dst = self.MIb[:, xo + kc * tt: xo + (kc + 1) * tt]
            eng = "pool" if kc % 2 == 0 else "act"
            if eng == "act":
                t.op("act", lambda e, s=src, d=dst: e.copy(d, s), reads=self.xr(ti, tt) + ["MI_own"], writes=["xbt"])
            else:
                t.op("pool", lambda e, s=src, d=dst: e.tensor_copy(d, s), reads=self.xr(ti, tt) + ["MI_own"], writes=["xbt"])

    def xr(self, ti, tt):
        return [("x32", (ti * tt) // TTM + i) for i in range(tt // TTM)]

    def tick(self, k=1):
        for _ in range(k):
            if not self.bg:
                return
            try:
                next(self.bg[0])
            except StopIteration:
                self.bg.pop(0)

    def drain(self, upto=None):
        while self.bg:
            if upto is not None and upto not in self.bg:
                return
            try:
                next(self.bg[0])
            except StopIteration:
                self.bg.pop(0)

    def ln_task(self, l, gcol, bcol, ti):
        t = self.t
        n = TTM
        t0 = ti * n
        xres = [("x32", ti)]
        for o in range(KC):
            z = self.x32[:, o * T + t0: o * T + t0 + n]
            zb = self.lnz[:, o * n:(o + 1) * n]
            zq = self.lnz[:, (KC + o) * n:(KC + o + 1) * n]
            t.op("dve", lambda e, z=z, zb=zb: e.tensor_copy(zb, z), reads=xres, writes=["lnzb"])
            t.op("act", lambda e, z=z, zq=zq: e.activation(out=zq, in_=z, func=AF.Square), reads=xres, writes=["lnzq"])
            yield
        for _ in range(3):
            yield
        bs = 7
        pm = [(self.ones[:], self.lnz[:, o * n:(o + 1) * n]) for o in range(KC)]
        pq = [(self.ones[:], self.lnz[:, (KC + o) * n:(KC + o + 1) * n]) for o in range(KC)]
        self.mm_group(self.ps[bs][:, 0:n], ("ps", bs), pm, ["ones", "lnzb"])
        yield
        self.mm_group(self.ps[bs][:, n:2 * n], ("ps", bs), pq, ["ones", "lnzq"])
        yield
        yield
        nm = self.negmean[:, 0:n]
        rs = self.rstd[:, 0:n]
        nr = self.nmr[:, 0:n]
        t.op("act", lambda e: e.mul(nm, self.ps[bs][:, 0:n], -1.0 / D), reads=[("ps", bs)], writes=["negmean"])
        yield
        t.op("act", lambda e: e.activation(out=nr, in_=nm, func=AF.Square), reads=["negmean"], writes=["nmr"])
        yield
        t.op("dve", lambda e: e.scalar_tensor_tensor(out=rs, in0=self.ps[bs][:, n:2 * n], scalar=1.0 / D, in1=nr,
                                                      op0=ALU.mult, op1=ALU.subtract),
             reads=[("ps", bs), "nmr"], writes=["rstd"])
        yield
        t.op("act", lambda e: e.activation(out=rs, in_=rs, func=AF.Ln, bias=EPS, scale=1.0), reads=["rstd"], writes=["rstd"])
        yield
        t.op("act", lambda e: e.activation(out=rs, in_=rs, func=AF.Exp, scale=-0.5), reads=["rstd"], writes=["rstd"])
        yield
        t.op("dve", lambda e: e.tensor_tensor(out=nr, in0=nm, in1=rs, op=ALU.mult), reads=["negmean", "rstd"], writes=["nmr"])
        yield
        for o in range(KC):
            z = self.x32[:, o * T + t0: o * T + t0 + n]
            g = self.par(l, gcol + o)
            bb = self.par(l, bcol + o)
            cr = ("x32c", ti, o)
            t.op("dve", lambda e, z=z: e.tensor_tensor(out=z, in0=z, in1=rs, op=ALU.mult), reads=xres + ["rstd"], writes=[cr])
            t.op("dve", lambda e, z=z: e.tensor_tensor(out=z, in0=z, in1=nr, op=ALU.add), reads=["nmr"], writes=[cr])
            yield
            t.op("act", lambda e, z=z, g=g, bb=bb: e.activation(out=z, in_=z, func=AF.Identity, bias=bb, scale=g),
                 reads=["params"], writes=[cr])
            yield
        t.op("act", lambda e: e.copy(self.scr[:, 1:2], self.scr[:, 2:3]), reads=[("x32c", ti, o) for o in range(KC)],
             writes=xres + ["scr1"])

    def outproj_residual(self, typ, ti, tt):
        t = self.t
        t0 = ti * tt
        nko = NKO[typ]
        for o in range(KC):
            b = self.bank(0, 7)
            pairs = []
            for i in range(nko):
                rows = 96 if (typ == 0 and i < 8) else 128
                pairs.append((self.wout(typ, i, o, rows), self.MIb[0:rows, self.o_mix + i * tt: self.o_mix + (i + 1) * tt]))
            self.mm_group(self.ps[b][:, 0:tt], ("ps", b), pairs, ["W2_own", ("wout", 0), ("wout", 1), "mixT", "MI_own"])
            z = self.x32[:, o * T + t0: o * T + t0 + tt]
            t.op("dve", lambda e, z=z, b=b: e.scalar_tensor_tensor(out=z, in0=z, scalar=ALPHA, in1=self.ps[b][:, 0:tt],
                                                                  op0=ALU.mult, op1=ALU.add),
                 reads=[("ps", b)] + self.xr(ti, tt), writes=self.xr(ti, tt))
            self.tick()

    def xattn_gen(self, typ, ti, tt, memq_c0, lo):
        t = self.t
        for hp in range(2):
            b = self.bank(lo, 7)
            self.inproj_fm(typ, memq_c0 + hp * 128, 128, tt, self.ps[b][:, 0:tt], ("ps", b))
            t.op("act", lambda e, b=b, hp=hp: e.copy(self.MIb[:, self.o_memq + hp * tt: self.o_memq + (hp + 1) * tt], self.ps[b][:, 0:tt]),
                 reads=[("ps", b), "MI_own"], writes=[("memq", hp)])
            yield

        def s_stage(h):
            hp, hh = h // 2, h % 2
            r0 = 64 * hh
            eb = h % 2
            for mc in range(2):
                b = self.bank(lo, 7)
                t.op("pe", lambda e, b=b, hp=hp, mc=mc, r0=r0: e.matmul(
                    self.ps[b][:, 0:tt],
                    self.memK[r0:r0 + 64, hp * NMEM + mc * 128: hp * NMEM + mc * 128 + 128],
                    self.MIb[r0:r0 + 64, self.o_memq + hp * tt: self.o_memq + (hp + 1) * tt],
                    start=True, stop=True),
                    reads=["memK", ("memq", hp), "MI_own"], writes=[("ps", b)])
                dst = self.MIb[:, self.o_expp + (eb * 2 + mc) * tt: self.o_expp + (eb * 2 + mc + 1) * tt]
                t.op("act", lambda e, b=b, dst=dst: e.activation(out=dst, in_=self.ps[b][:, 0:tt], func=AF.Exp, scale=0.125),
                     reads=[("ps", b), "MI_own"], writes=[("expp", eb, mc)])

        def pv_stage(h):
            hp, hh = h // 2, h % 2
            r0 = 64 * hh
            eb = h % 2
            bo = self.bank(lo, 7)
            bs = self.bank(lo, 7)
            pairs_o, pairs_s = [], []
            for mc in range(2):
                ep = self.MIb[:, self.o_expp + (eb * 2 + mc) * tt: self.o_expp + (eb * 2 + mc + 1) * tt]
                pairs_o.append((self.memV[:, mc * 256 + hp * 128: mc * 256 + hp * 128 + 128], ep))
                pairs_s.append((self.ones[:], ep))
            rd = ["memV", "ones", ("expp", eb, 0), ("expp", eb, 1), "MI_own"]
            self.mm_group(self.ps[bo][:, 0:tt], ("ps", bo), pairs_o, rd)
            self.mm_group(self.ps[bs][:, 0:tt], ("ps", bs), pairs_s, rd)
            rc = self.rcb[r0:r0 + 64, 0:tt]
            t.op("dve", lambda e, bs=bs, rc=rc, r0=r0: e.reciprocal(rc, self.ps[bs][r0:r0 + 64, 0:tt]),
                 reads=[("ps", bs)], writes=["rcb"])
            i = NKO[typ] - 2 + hp
            dst = self.MIb[r0:r0 + 64, self.o_mix + i * tt: self.o_mix + (i + 1) * tt]
            t.op("dve", lambda e, bo=bo, rc=rc, dst=dst, r0=r0: e.tensor_tensor(out=dst, in0=self.ps[bo][r0:r0 + 64, 0:tt], in1=rc, op=ALU.mult),
                 reads=[("ps", bo), "rcb", "MI_own"], writes=["mixT"])

        s_stage(0)
        yield
        for h in range(4):
            if h + 1 < 4:
                s_stage(h + 1)
                yield
            pv_stage(h)
            self.tick()
            yield

    def fill(self, k=1):
        for _ in range(k):
            if self.filler is None:
                return
            try:
                next(self.filler)
            except StopIteration:
                self.filler = None

    def fill_all(self):
        while self.filler is not None:
            self.fill()

    def conv_tile(self, l, ti, tt):
        t = self.t
        typ = 1
        PC = 208

        def stage1(ci):
            p = ci % 2
            hsn, chn = ("hs", p), ("ch", p)
            bh = self.bank(0, 7)
            self.inproj_fm(typ, 1536 + 128 * ci, 128, tt, self.ps[bh][:, 0:tt], ("ps", bh))
            hs = self.MIf[:, self.f_hs[p]:self.f_hs[p] + tt]
            t.op("act", lambda e, bh=bh, hs=hs: e.copy(hs, self.ps[bh][:, 0:tt]), reads=[("ps", bh), "MI_own"], writes=[hsn])
            bc = self.bank(0, 7)
            self.inproj_fm(typ, 768 + 128 * ci, 128, tt, self.ps[bc][:, 0:tt], ("ps", bc))
            ch = self.MIf[:, self.f_ch[p]:self.f_ch[p] + tt + 2]
            t.op("act", lambda e, ci=ci, ch=ch: e.copy(ch[:, 0:2], self.halo[:, 2 * ci:2 * ci + 2]),
                 reads=[("halo", ci), "MI_own"], writes=[chn])
            t.op("dve", lambda e, bc=bc, hs=hs, ch=ch: e.tensor_tensor(out=ch[:, 2:tt + 2], in0=self.ps[bc][:, 0:tt], in1=hs, op=ALU.mult),
                 reads=[("ps", bc), hsn, "MI_own"], writes=[chn])

        def stage2(ci):
            p = ci % 2
            chn = ("ch", p)
            an = "tmpA" if p == 0 else "tmpB"
            ch = self.MIf[:, self.f_ch[p]:self.f_ch[p] + tt + 2]
            t.op("act", lambda e, ci=ci, ch=ch: e.copy(self.halo[:, 2 * ci:2 * ci + 2], ch[:, tt:tt + 2]),
                 reads=[chn], writes=[("halo", ci)])
            a = (self.tmpA if p == 0 else self.tmpB)[:, 0:tt]
            w0, w1, w2 = (self.par(l, PC + ci * 3 + k) for k in range(3))
            t.op("act", lambda e, a=a, ch=ch, w2=w2: e.mul(a, ch[:, 2:tt + 2], w2), reads=[chn, "params"], writes=[an])
            t.op("dve", lambda e, a=a, ch=ch, w1=w1: e.scalar_tensor_tensor(out=a, in0=ch[:, 1:tt + 1], scalar=w1, in1=a, op0=ALU.mult, op1=ALU.add),
                 reads=[chn, "params", an], writes=[an])
            t.op("dve", lambda e, a=a, ch=ch, w0=w0: e.scalar_tensor_tensor(out=a, in0=ch[:, 0:tt], scalar=w0, in1=a, op0=ALU.mult, op1=ALU.add),
                 reads=[chn, "params", an], writes=[an])
            bb = self.bank(0, 7)
            self.inproj_fm(typ, 128 * ci, 128, tt, self.ps[bb][:, 0:tt], ("ps", bb))
            dst = self.MIb[:, self.o_mix + ci * tt: self.o_mix + (ci + 1) * tt]
            t.op("dve", lambda e, bb=bb, a=a, dst=dst: e.tensor_tensor(out=dst, in0=self.ps[bb][:, 0:tt], in1=a, op=ALU.mult),
                 reads=[("ps", bb), an, "MI_own"], writes=["mixT"])
            self.tick(5)

        stage1(0)
        for ci in range(6):
            if ci + 1 < 6:
                stage1(ci + 1)
            stage2(ci)

    def gla_tile(self, l, ti, tt):
        t = self.t
        typ = 0
        nch = tt // 128
        triF = self.consts[:, 0:128]
        triR = self.consts[:, 128:256]
        maskT = self.consts[:, 256:384]
        PH = 226

        def gps(h):
            return self.ps[h // 2][0:96, (h % 2) * tt:(h % 2 + 1) * tt]
        b = self.bank(2, 4)
        self.inproj_fm(typ, 2304, 16, tt, self.ps[b][0:16, 0:tt], ("ps", b))
        xg = self.MIf[0:17, self.f_xg:self.f_xg + tt]
        t.op("act", lambda e, b=b: e.copy(self.MIf[0:16, self.f_xg:self.f_xg + tt], self.ps[b][0:16, 0:tt]),
             reads=[("ps", b), "MI_own"], writes=["xg"])
        ltok = self.MIf[:, self.f_ltok:self.f_ltok + 384]
        eend = self.MIf[:, self.f_eend:self.f_eend + 384]
        for c in range(nch):
            b = self.bank(2, 4)
            t.op("pe", lambda e, b=b, c=c: e.matmul(self.ps[b][:, 0:384], xg[:, c * 128:(c + 1) * 128], self.wa2[:, :], start=True, stop=True),
                 reads=["xg", "wa2", "MI_own"], writes=[("ps", b)])
            t.op("act", lambda e, b=b: e.activation(out=ltok, in_=self.ps[b][:, 0:384], func=AF.Exp, scale=-1.0),
                 reads=[("ps", b), "MI_own"], writes=["ltok"])
            t.op("act", lambda e: e.activation(out=ltok, in_=ltok, func=AF.Ln, bias=1.0, scale=1.0), reads=["ltok"], writes=["ltok"])
            for half in range(2):
                b4 = self.bank(4, 7)
                self.inproj_tm(typ, 768 + 384 * half, 384, tt, c, self.ps[b4][:, 0:384], ("ps", b4))
                vd = self.MIb[:, self.o_vtok + c * 768 + half * 384: self.o_vtok + c * 768 + (half + 1) * 384]
                t.op("act", lambda e, b4=b4, vd=vd: e.copy(vd, self.ps[b4][:, 0:384]),
                     reads=[("ps", b4), "MI_own"], writes=[("vtok", c)])
            for h in range(4):
                t.op("pe", lambda e, h=h, c=c: e.matmul(gps(h)[:, c * 128:(c + 1) * 128], ltok[:, 96 * h:96 * h + 96], triF, start=True, stop=True),
                     reads=["ltok", "consts", "MI_own"], writes=[("ps", h // 2)])
            b2 = self.bank(2, 4)
            t.op("pe", lambda e, b2=b2: e.matmul(self.ps[b2][:, 0:384], triR, ltok, start=True, stop=True),
                 reads=["ltok", "consts", "MI_own"], writes=[("ps", b2)])
            t.op("act", lambda e, b2=b2: e.activation(out=eend, in_=self.ps[b2][:, 0:384], func=AF.Exp),
                 reads=[("ps", b2), "MI_own"], writes=["eend"])
            b3 = self.bank(4, 7)
            self.inproj_tm(typ, 384, 384, tt, c, self.ps[b3][:, 0:384], ("ps", b3))
            kd = self.MIb[:, self.o_kend + c * 384: self.o_kend + (c + 1) * 384]
            t.op("dve", lambda e, b3=b3, kd=kd: e.tensor_tensor(out=kd, in0=self.ps[b3][:, 0:384], in1=eend, op=ALU.mult),
                 reads=[("ps", b3), "eend", "MI_own"], writes=[("kend", c)])
        et = self.MIf[0:96, self.f_et:self.f_et + tt]
        for h in range(4):
            bq = self.bank(4, 7)
            self.inproj_fm(typ, 96 * h, 96, tt, self.ps[bq][0:96, 0:tt], ("ps", bq))
            bk = self.bank(4, 7)
            self.inproj_fm(typ, 384 + 96 * h, 96, tt, self.ps[bk][0:96, 0:tt], ("ps", bk))
            t.op("act", lambda e, h=h: e.activation(out=et, in_=gps(h), func=AF.Exp),
                 reads=[("ps", h // 2), "MI_own"], writes=["et"])
            dec = self.MIf[0:96, self.f_decay + h * 4: self.f_decay + h * 4 + nch]
            t.op("dve", lambda e, dec=dec: e.tensor_copy(dec, et[:, 127:tt:128]), reads=["et"], writes=[("decay", h)])
            qd = self.MIb[0:96, self.o_qd + h * tt: self.o_qd + (h + 1) * tt]
            t.op("dve", lambda e, bq=bq, qd=qd: e.scalar_tensor_tensor(out=qd, in0=self.ps[bq][0:96, 0:tt], scalar=QSCALE, in1=et,
                                                                       op0=ALU.mult, op1=ALU.mult),
                 reads=[("ps", bq), "et", "MI_own"], writes=[("qd", h)])
            einv = self.tmpD[0:96, 0:tt]
            t.op("dve", lambda e, einv=einv: e.reciprocal(einv, et), reads=["et"], writes=["tmpD"])
            ki = self.MIb[0:96, self.o_ki + h * tt: self.o_ki + (h + 1) * tt]
            t.op("dve", lambda e, bk=bk, ki=ki, einv=einv: e.tensor_tensor(out=ki, in0=self.ps[bk][0:96, 0:tt], in1=einv, op=ALU.mult),
                 reads=[("ps", bk), "tmpD", "MI_own"], writes=[("ki", h)])
        self.filler = self.xattn_gen(typ, ti, tt, 2320, 2)
        tmps = [(self.tmpA, "tmpA"), (self.tmpB, "tmpB"), (self.tmpC, "tmpC"), (self.tmpD, "tmpD")]
        for hpair in range(2):
            heads = (2 * hpair, 2 * hpair + 1)
            srs = {}
            for h in heads:
                for vt in range(2):
                    i8 = 2 * h + vt
                    br = self.bank(4, 7)
                    self.inproj_fm(typ, 1536 + 96 * i8, 96, tt, self.ps[br][0:96, 0:tt], ("ps", br))
                    tb, tn = tmps[(h % 2) * 2 + vt]
                    sr = tb[0:96, 0:tt]
                    t.op("act", lambda e, br=br, sr=sr: e.activation(out=sr, in_=self.ps[br][0:96, 0:tt], func=AF.Silu),
                         reads=[("ps", br)], writes=[tn])
                    srs[(h, vt)] = (sr, tn)
            self.fill()
            for c in range(nch):
                gchunk = ti * nch + c
                cur = gchunk % 2
                nxt = 1 - cur
                cs = slice(c * 128, (c + 1) * 128)
                bas, bkvs = {}, {}
                for h in heads:
                    qd = self.MIb[0:96, self.o_qd + h * tt: self.o_qd + (h + 1) * tt]
                    ki = self.MIb[0:96, self.o_ki + h * tt: self.o_ki + (h + 1) * tt]
                    ba = self.bank(2, 4)
                    t.op("pe", lambda e, ba=ba, cs=cs, ki=ki, qd=qd: e.matmul(self.ps[ba][:, 0:128], ki[:, cs], qd[:, cs], start=True, stop=True),
                         reads=[("ki", h), ("qd", h), "MI_own"], writes=[("ps", ba)])
                    bkv = self.bank(4, 7)
                    t.op("pe", lambda e, bkv=bkv, c=c, h=h: e.matmul(
                        self.ps[bkv][0:96, 0:192], self.MIb[:, self.o_kend + c * 384 + 96 * h: self.o_kend + c * 384 + 96 * h + 96],
                        self.MIb[:, self.o_vtok + c * 768 + h * 192: self.o_vtok + c * 768 + (h + 1) * 192], start=True, stop=True),
                        reads=[("kend", c), ("vtok", c), "MI_own"], writes=[("ps", bkv)])
                    bas[h], bkvs[h] = ba, bkv
                for h in heads:
                    qd = self.MIb[0:96, self.o_qd + h * tt: self.o_qd + (h + 1) * tt]
                    pb = h % 2
                    st32 = self.MIf[0:96, self.f_state + h * 192: self.f_state + (h + 1) * 192]
                    stb_cur = self.MIb[0:96, self.o_stb + (h * 2 + cur) * 192: self.o_stb + (h * 2 + cur + 1) * 192]
                    stb_nxt = self.MIb[0:96, self.o_stb + (h * 2 + nxt) * 192: self.o_stb + (h * 2 + nxt + 1) * 192]
                    ba, bkv = bas[h], bkvs[h]
                    ab = (h % 2) * 2 + c % 2
                    att = self.MIb[:, self.o_att + ab * 128: self.o_att + (ab + 1) * 128]
                    t.op("dve", lambda e, ba=ba, att=att: e.tensor_tensor(out=att, in0=self.ps[ba][:, 0:128], in1=maskT, op=ALU.mult),
                         reads=[("ps", ba), "consts", "MI_own"], writes=[("att", ab)])
                    dcol = self.f_decay + h * 4 + c
                    t.op("dve", lambda e, bkv=bkv, st32=st32, dcol=dcol: e.scalar_tensor_tensor(
                        out=st32, in0=st32, scalar=self.MIf[0:96, dcol:dcol + 1], in1=self.ps[bkv][0:96, 0:192], op0=ALU.mult, op1=ALU.add),
                        reads=[("ps", bkv), ("decay", h), ("state", h), "MI_own"], writes=[("state", h)])
                    for vt in range(2):
                        vcol = self.o_vtok + c * 768 + h * 192 + vt * 96
                        osl = slice(vt * tt + c * 128, vt * tt + (c + 1) * 128)
                        t.op("pe", lambda e, vcol=vcol, att=att, osl=osl, pb=pb: e.matmul(
                            self.ps[pb][0:96, osl], self.MIb[:, vcol:vcol + 96], att, start=True, stop=False),
                            reads=[("vtok", c), ("att", ab), "MI_own"], writes=[("ps", pb)], sig=False)
                        t.op("pe", lambda e, vt=vt, cs=cs, osl=osl, stb_cur=stb_cur, qd=qd, pb=pb: e.matmul(
                            self.ps[pb][0:96, osl], stb_cur[:, vt * 96:(vt + 1) * 96], qd[:, cs], start=False, stop=True),
                            reads=[("stb", h, cur), ("qd", h), "MI_own"], writes=[("ps", pb)])
                    t.op("act", lambda e, st32=st32, stb_nxt=stb_nxt: e.copy(stb_nxt, st32),
                         reads=[("state", h), "MI_own"], writes=[("stb", h, nxt)])
                self.tick()
                self.fill()
            sqs, rrs, bsss = {}, {}, {}
            for h in heads:
                k = h % 2
                sqs[h] = (self.MIb[0:96, (self.o_sq if k == 0 else self.o_sq2): (self.o_sq if k == 0 else self.o_sq2) + 2 * tt], ("sq", k))
                rrs[h] = (self.MIf[0:96, (self.f_rrms if k == 0 else self.f_rrms2): (self.f_rrms if k == 0 else self.f_rrms2) + tt], ("rrms", k))
                bsss[h] = self.bank(2, 4)
            for h in heads:
                pb = h % 2
                sq, sqn = sqs[h]
                t.op("act", lambda e, sq=sq, pb=pb: e.activation(out=sq, in_=self.ps[pb][0:96, 0:2 * tt], func=AF.Square),
                     reads=[("ps", pb), "MI_own"], writes=[sqn])
            for h in heads:
                sq, sqn = sqs[h]
                bss = bsss[h]
                for vt in range(2):
                    t.op("pe", lambda e, vt=vt, bss=bss, sq=sq: e.matmul(self.ps[bss][0:96, 0:tt], self.ones[0:96, 0:96], sq[:, vt * tt:(vt + 1) * tt], start=(vt == 0), stop=(vt == 1)),
                         reads=[sqn, "ones", "MI_own"], writes=[("ps", bss)], sig=(vt == 1))
            for h in heads:
                rr, rrn = rrs[h]
                bss = bsss[h]
                t.op("act", lambda e, bss=bss, rr=rr: e.activation(out=rr, in_=self.ps[bss][0:96, 0:tt], func=AF.Ln, bias=EPS, scale=1.0 / 192.0),
                     reads=[("ps", bss), "MI_own"], writes=[rrn])
            for h in heads:
                rr, rrn = rrs[h]
                t.op("act", lambda e, rr=rr: e.activation(out=rr, in_=rr, func=AF.Exp, scale=-0.5), reads=[rrn], writes=[rrn])
            self.fill()
            for h in heads:
                pb = h % 2
                rr, rrn = rrs[h]
                for vt in range(2):
                    i8 = 2 * h + vt
                    sr, srn = srs[(h, vt)]
                    hg = self.par(l, PH + i8, rows=96)
                    t.op("dve", lambda e, sr=sr, hg=hg, rr=rr: e.scalar_tensor_tensor(out=sr, in0=sr, scalar=hg, in1=rr, op0=ALU.mult, op1=ALU.mult),
                         reads=[srn, rrn, "params"], writes=[srn])
                    dst = self.MIb[0:96, self.o_mix + i8 * tt: self.o_mix + (i8 + 1) * tt]
                    t.op("dve", lambda e, vt=vt, sr=sr, dst=dst, pb=pb: e.tensor_tensor(out=dst, in0=self.ps[pb][0:96, vt * tt:(vt + 1) * tt], in1=sr, op=ALU.mult),
                         reads=[("ps", pb), srn, "MI_own"], writes=["mixT"])
            self.tick()
            self.fill()

    def mixer(self, l):
        t = self.t
        typ = l % 2
        tt = TTM if typ == 0 else 512
        for k, v in self.lay[typ].items():
            setattr(self, k, v)
        if getattr(self, "memkv_done", None) != l:
            self.mem_kv(l)
        if getattr(self, "wout_pending", None) == l:
            self.wout_pending = None
            self.load_wout(l)
        if typ == 0:
            t.op("dve", lambda e: e.memset(self.MIf[0:96, self.f_state:self.f_state + 768], 0.0),
                 reads=["MI_own"], writes=[("state", h) for h in range(4)])
            t.op("dve", lambda e: e.memset(self.MIb[0:96, self.o_stb:self.o_stb + 1536], 0.0),
                 reads=["MI_own"], writes=[("stb", h, k) for h in range(4) for k in range(2)])
            t.op("dve", lambda e: e.memset(self.MIf[0:17, self.f_xg:self.f_xg + tt], 1.0), reads=["MI_own"], writes=["xg"])
        else:
            t.op("dve", lambda e: e.memset(self.halo[:], 0.0), writes=[("halo", ci) for ci in range(6)])
        for ti in range(T // tt):
            if self.ln2_tasks:
                self.drain(upto=self.ln2_tasks[(ti + 1) * tt // TTM - 1])
            self.cast_tile(ti, tt)
            if typ == 0:
                self.gla_tile(l, ti, tt)
            else:
                self.filler = self.xattn_gen(typ, ti, tt, 2304, 0)
                self.conv_tile(l, ti, tt)
            self.fill_all()
            self.outproj_residual(typ, ti, tt)
            for i in range(tt // TTM):
                self.bg.append(self.ln_task(l, 0, 8, ti * tt // TTM + i))
        self.drain()

    def load_wup(self, l, j, buf):
        self.t.dma("pool", self.W2[:, buf * 2048:(buf + 1) * 2048], self.wup_d[l, j],
                   reads=[], writes=[("wupb", buf)] + (["W2_own"] if self.first_ffn_dma else []))
        self.first_ffn_dma = False

    def ffn(self, l, next_l):
        t = self.t
        tt = TTF
        ntile = T // tt
        PW, PB = 32, 164
        self.first_ffn_dma = True
        nwb = 3
        units = [(gi, jj) for gi, (s0, n) in enumerate(GROUPS) for jj in range(n)]
        for k in range(nwb - 1):
            gi, jj = units[k]
            self.load_wup(l, GROUPS[gi][0] + jj, k % nwb)
        self.barrier(["MI_own"])
        xr = [("x32", i) for i in range(T // TTM)]
        k = 0
        for hf in range(2):
            for kc in range(KC):
                src = self.x32[:, kc * T + hf * 1024: kc * T + (hf + 1) * 1024]
                dst = self.MIb[:, kc * T + hf * 1024: kc * T + (hf + 1) * 1024]
                xrh = [("x32", i) for i in range(hf * 1024 // TTM, (hf + 1) * 1024 // TTM)]
                if k % 2 == 0:
                    t.op("act", lambda e, s=src, d=dst: e.copy(d, s), reads=xrh + ["MI_own"], writes=[("xbf", kc, hf)])
                else:
                    t.op("dve", lambda e, s=src, d=dst: e.tensor_copy(d, s), reads=xrh + ["MI_own"], writes=[("xbf", kc, hf)])
                k += 1
        ug = self.MIf[:, self.f_ug:self.f_ug + 2 + T]
        uv = self.MIf[:, self.f_uv:self.f_uv + 2 + T]
        t.op("dve", lambda e: e.memset(ug[:, 0:2], 0.0), reads=["MI_own"], writes=[("ug", -1)])
        t.op("dve", lambda e: e.memset(uv[:, 0:2], 0.0), reads=["MI_own"], writes=[("uv", -1)])
        agb = [(self.tmpA, "tmpA"), (self.tmpC, "tmpC")]
        avb = [(self.tmpB, "tmpB"), (self.tmpD, "tmpD")]
        pend = []
        ucount = 0
        WDN0 = 6144
        last_gi = len(GROUPS) - 1

        def stage_b(item):
            jj, t0, p = item
            ag, agn = agb[p]
            av, avn = avb[p]
            t.op("act", lambda e, ag=ag: e.activation(out=ag[:, 0:tt], in_=ag[:, 0:tt], func=AF.Silu), reads=[agn], writes=[agn])
            hdst = self.U[:, jj * T + t0: jj * T + t0 + tt]
            t.op("pool", lambda e, ag=ag, av=av, hdst=hdst: e.tensor_tensor(out=hdst, in0=ag[:, 0:tt], in1=av[:, 0:tt], op=ALU.mult),
                 reads=[agn, avn], writes=self.ublk(jj * T + t0, jj * T + t0 + tt))

        def wdn_names(slot_off, n):
            return [("wdn", q) for q in range(slot_off // 1024, (slot_off + n * 128 - 1) // 1024 + 1)]

        def load_wdn(gi, o, slot_off, n):
            s0 = GROUPS[gi][0]
            t.dma("pool", self.W2[:, WDN0 + slot_off: WDN0 + slot_off + n * 128], self.wdn_d[l, o, :, s0 * 128:(s0 + n) * 128],
                  reads=[], writes=wdn_names(slot_off, n))

        def wdn_names(slot_off, n):
            return [("wdn", q) for q in range(slot_off // 1024, (slot_off + n * 128 - 1) // 1024 + 1)]

        for ui, (gi, jj) in enumerate(units):
            s0, gn = GROUPS[gi]
            j = s0 + jj
            buf = ui % nwb
            if ui + nwb - 1 < len(units):
                g2, jj2 = units[ui + nwb - 1]
                self.load_wup(l, GROUPS[g2][0] + jj2, (ui + nwb - 1) % nwb)
            if jj == 0:
                if gi < last_gi:
                    load_wdn(gi, 0, 0, gn)
                    load_wdn(gi, 1, 2048, gn)
                else:
                    for o in range(KC):
                        load_wdn(gi, o, o * gn * 128, gn)
            wg = [self.par(l, PW + j * 3 + k) for k in range(3)]
            wv = [self.par(l, PW + (22 + j) * 3 + k) for k in range(3)]
            bgp = self.par(l, PB + j)
            bvp = self.par(l, PB + 22 + j)
            for ti in range(ntile):
                t0 = ti * tt
                p = ucount % 2
                ucount += 1
                bg = self.bank(0, 7)
                bv = self.bank(0, 7)
                xbf_res = [("xbf", kc, t0 // 1024) for kc in range(KC)] + ["MI_own"]
                for which, bnk in ((0, bg), (1, bv)):
                    pairs = [(self.W2[:, buf * 2048 + kc * 256 + which * 128: buf * 2048 + kc * 256 + which * 128 + 128],
                              self.MIb[:, kc * T + t0: kc * T + t0 + tt]) for kc in range(KC)]
                    self.mm_group(self.ps[bnk][:, 0:tt], ("ps", bnk), pairs, [("wupb", buf), "W2_own"] + xbf_res)
                ag, agn = agb[p]
                av, avn = avb[p]
                paths = ((ug, "ug", bg, ag[:, 0:tt], agn, wg, bgp), (uv, "uv", bv, av[:, 0:tt], avn, wv, bvp))
                for (u, un, bnk, a, ares, w, bp) in paths:
                    t.op("act", lambda e, u=u, bnk=bnk: e.copy(u[:, 2 + t0:2 + t0 + tt], self.ps[bnk][:, 0:tt]),
                         reads=[("ps", bnk), "MI_own"], writes=[(un, ti)])
                if pend:
                    stage_b(pend.pop(0))
                for (u, un, bnk, a, ares, w, bp) in paths:
                    ur = [(un, ti), (un, ti - 1)]
                    t.op("act", lambda e, a=a, bnk=bnk, w=w, bp=bp: e.activation(out=a, in_=self.ps[bnk][:, 0:tt], func=AF.Identity, bias=bp, scale=w[2]),
                         reads=[("ps", bnk), "params"], writes=[ares])
                    t.op("dve", lambda e, a=a, u=u, w=w: e.scalar_tensor_tensor(out=a, in0=u[:, 1 + t0:1 + t0 + tt], scalar=w[1], in1=a, op0=ALU.mult, op1=ALU.add),
                         reads=ur + ["params", ares, "MI_own"], writes=[ares])
                    t.op("dve", lambda e, a=a, u=u, w=w: e.scalar_tensor_tensor(out=a, in0=u[:, t0:t0 + tt], scalar=w[0], in1=a, op0=ALU.mult, op1=ALU.add),
                         reads=ur + ["params", ares, "MI_own"], writes=[ares])
                pend.append((jj, t0, p))
            if jj == gn - 1:
                while pend:
                    stage_b(pend.pop(0))
                if gi < last_gi:
                    for o in range(KC):
                        so = (o % 2) * 2048
                        for ti in range(ntile):
                            t0 = ti * tt
                            b = self.bank(0, 7)
                            pairs = [(self.W2[:, WDN0 + so + q * 128: WDN0 + so + (q + 1) * 128], self.U[:, q * T + t0: q * T + t0 + tt]) for q in range(gn)]
                            hres = []
                            for q in range(gn):
                                hres += self.ublk(q * T + t0, q * T + t0 + tt)
                            self.mm_group(self.ps[b][:, 0:tt], ("ps", b), pairs, wdn_names(so, gn) + ["W2_own"] + hres)
                            z = self.x32[:, o * T + t0: o * T + t0 + tt]
                            xres = [("x32", t0 // TTM + i) for i in range(tt // TTM)]
                            if gi == 0:
                                t.op("dve", lambda e, z=z, b=b: e.scalar_tensor_tensor(out=z, in0=z, scalar=ALPHA, in1=self.ps[b][:, 0:tt], op0=ALU.mult, op1=ALU.add),
                                     reads=[("ps", b)] + xres, writes=xres)
                            else:
                                t.op("dve", lambda e, z=z, b=b: e.tensor_tensor(out=z, in0=z, in1=self.ps[b][:, 0:tt], op=ALU.add),
                                     reads=[("ps", b)] + xres, writes=xres)
                        if o + 2 < KC:
                            load_wdn(gi, o + 2, so, gn)
                else:
                    self.barrier(["MI_own"])
                    if next_l is not None:
                        self.o_wkv = self.lay[next_l % 2]["o_wkv"]
                        self.mem_kv(next_l)
                        self.memkv_done = next_l
                        self.load_win(next_l, [2, 3])
                    self.ln2_tasks = []
                    t2 = TTM
                    for ti in range(T // t2):
                        t0 = ti * t2
                        xres = [("x32", ti)]
                        hres = []
                        for q in range(gn):
                            hres += self.ublk(q * T + t0, q * T + t0 + t2)
                        for o in range(KC):
                            so = o * gn * 128
                            b = self.bank(0, 7)
                            pairs = [(self.W2[:, WDN0 + so + q * 128: WDN0 + so + (q + 1) * 128], self.U[:, q * T + t0: q * T + t0 + t2]) for q in range(gn)]
                            self.mm_group(self.ps[b][:, 0:t2], ("ps", b), pairs, wdn_names(so, gn) + ["W2_own"] + hres)
                            z = self.x32[:, o * T + t0: o * T + t0 + t2]
                            t.op("dve", lambda e, z=z, b=b: e.tensor_tensor(out=z, in0=z, in1=self.ps[b][:, 0:t2], op=ALU.add),
                                 reads=[("ps", b)] + xres, writes=xres)
                            self.tick(5)
                        task = self.ln_task(l, 16, 24, ti)
                        self.ln2_tasks.append(task)
                        self.bg.append(task)
                    if next_l is not None:
                        self.load_win(next_l, [0, 1])
                        self.load_wout(next_l)

    def build(self):
        t = self.t
        gl = [l for l in self.layers if l % 2 == 0]
        self.first_gla = gl[0] if gl else -1
        self.ln2_tasks = []
        self.prologue()
        self.load_win(self.layers[0], [0, 1, 2, 3])
        for (dst, src, q) in self.x_rest:
            t.dma("sp", dst, src, reads=self.ublk(0, USZ), writes=[("x32", q)])
        self.wout_pending = self.layers[0]
        for li, l in enumerate(self.layers):
            nl = self.layers[li + 1] if li + 1 < len(self.layers) else None
            self.mixer(l)
            self.ffn(l, nl)
        self.drain()
        yv = self.yT.rearrange("(kc p) t -> p kc t", p=128)
        x3 = self.x32[:].rearrange("p (kc t) -> p kc t", kc=KC)
        for q in range(4):
            t.dma("sp", yv[:, :, q * 512:(q + 1) * 512], x3[:, :, q * 512:(q + 1) * 512],
                  reads=[("x32", q * 512 // TTM + i) for i in range(512 // TTM)], writes=[("y", q)])
        t.wait_all("sp", [("y", q) for q in range(4)])
        return self.nc


def _prep_weights(inp):
    f = np.float32
    out = {}
    tri = np.arange(128)
    c = np.zeros((128, 384), f)
    c[:, 0:128] = np.where(tri[:, None] <= tri[None, :], -1.0 / 16.0, 0.0)
    c[:, 128:256] = np.where(tri[:, None] > tri[None, :], -1.0 / 16.0, 0.0)
    c[:, 256:384] = np.where(tri[:, None] <= tri[None, :], 1.0, 0.0)
    out["consts"] = c
    par = np.zeros((128, 4 * NPAR), f)
    for l in range(4):
        b = l * NPAR
        j = l // 2
        for name, col in (("ln1_g", 0), ("ln1_b", 8), ("ln2_g", 16), ("ln2_b", 24)):
            par[:, b + col:b + col + 8] = np.asarray(inp[name][l]).reshape(8, 128).T
        cw = np.asarray(inp["ffn_conv_w"][l])
        par[:, b + 32:b + 32 + 132] = cw.reshape(3, 44, 128).transpose(2, 1, 0).reshape(128, 132)
        par[:, b + 164:b + 208] = np.asarray(inp["ffn_conv_b"][l]).reshape(44, 128).T
        if l % 2 == 1:
            mw = np.asarray(inp["conv_w"][j])
            par[:, b + 208:b + 226] = mw.reshape(3, 6, 128).transpose(2, 1, 0).reshape(128, 18)
        else:
            par[0:96, b + 226:b + 234] = np.asarray(inp["gla_head_g"][j]).reshape(8, 96).T
    out["params"] = par

    def kmajor(w):
        L, K, N = w.shape
        return np.ascontiguousarray(w.reshape(L, K // 128, 128, N).transpose(0, 2, 1, 3).reshape(L, 128, (K // 128) * N))
    out["win_g"] = kmajor(np.asarray(inp["gla_w_in"], f))
    out["win_c"] = kmajor(np.asarray(inp["conv_w_in"], f))
    out["wkv"] = kmajor(np.asarray(inp["w_mem_kv"], f))
    out["wout_c"] = kmajor(np.asarray(inp["conv_w_out"], f))
    gw = np.asarray(inp["gla_w_out"], f)
    wg = np.zeros((2, 128, 10, 1024), f)
    wg[:, 0:96, 0:8, :] = gw[:, 0:768, :].reshape(2, 8, 96, 1024).transpose(0, 2, 1, 3)
    wg[:, :, 8:10, :] = gw[:, 768:1024, :].reshape(2, 2, 128, 1024).transpose(0, 2, 1, 3)
    out["wout_g"] = wg.reshape(2, 128, 10 * 1024)
    out["wa2"] = np.concatenate([np.asarray(inp["gla_w_a2"], f), np.asarray(inp["gla_b_a"], f)[:, None, :]], axis=1)
    wu = np.asarray(inp["ffn_w_up"], f).reshape(4, 8, 128, 2, 22, 128)
    out["wup"] = np.ascontiguousarray(wu.transpose(0, 4, 2, 1, 3, 5).reshape(4, 22, 128, 8 * 256))
    wd = np.asarray(inp["ffn_w_down"], f).reshape(4, 22, 128, 8, 128)
    out["wdn"] = np.ascontiguousarray(wd.transpose(0, 3, 2, 1, 4).reshape(4, 8, 128, 22 * 128))
    return out


_CACHE = {}


def _get_nc(layers):
    key = tuple(layers)
    if key not in _CACHE:
        _CACHE[key] = Builder(layers).build()
    return _CACHE[key]


def _run(layers, xT_list, memT_list, w):
    nc = _get_nc(layers)
    in_maps = []
    for c in range(8):
        m = dict(w)
        m["xT"] = xT_list[c]
        m["memT"] = memT_list[c]
        in_maps.append(m)
    res = run_bass_kernel_spmd(nc, in_maps, core_ids=list(range(8)))
    return [np.asarray(r["yT"]) for r in res.results]


LAUNCH_GROUPS = [[0, 1, 2, 3]]


def kernel(**inputs):
    x = np.asarray(inputs["x"], np.float32)
    mem = np.asarray(inputs["mem"], np.float32)
    w = _prep_weights(inputs)
    xT = [np.ascontiguousarray(x[b].T) for b in range(8)]
    memT = [np.ascontiguousarray(mem[b].T) for b in range(8)]
    for grp in LAUNCH_GROUPS:
        xT = _run(grp, xT, memT, w)
    return np.stack([np.ascontiguousarray(y.T) for y in xT], axis=0).astype(np.float32)
```

```python
import numpy as np
from contextlib import ExitStack
import concourse.bass as bass
import concourse.mybir as mybir
from concourse.bass_utils import run_bass_kernel_spmd

F32 = mybir.dt.float32
BF16 = mybir.dt.bfloat16
AF = mybir.ActivationFunctionType
ALU = mybir.AluOpType

T = 2048
D = 1024
KC = 8
NMEM = 256
TTM = 256
TTF = 512
NPAR = 240
ALPHA = float(8.0 ** 0.25)
EPS = 1e-5
QSCALE = float(96.0 ** -0.5)
GROUPS = [(0, 9), (9, 9), (18, 4)]
USZ = 20608
NDMA = 28
NDMA_SP = 8
NIN = {0: 2576, 1: 2560}
NKO = {0: 10, 1: 8}


class Trk:
    def __init__(self, nc, es):
        self.nc = nc
        self.engs = {"pe": nc.tensor, "act": nc.scalar, "dve": nc.vector,
                     "pool": nc.gpsimd, "sp": nc.sync}
        self.sems = {k: es.enter_context(nc.semaphore("c_" + k)) for k in self.engs}
        self.cnt = {k: 0 for k in self.engs}
        self.seen = {k: {} for k in self.engs}
        self.lastw = {}
        self.readers = {}
        self.dsem = [es.enter_context(nc.semaphore("d%d" % i)) for i in range(NDMA)]
        self.dval = [0] * NDMA
        self.dnext = {}
        self.nops = 0

    def _semh(self, key):
        if isinstance(key, tuple):
            return self.dsem[key[1]]
        return self.sems[key]

    def _wait(self, eng, tok):
        key, val = tok
        if key == "pe" and eng == "pe":
            return
        if self.seen[eng].get(key, 0) >= val:
            return
        self.seen[eng][key] = val
        self.engs[eng].wait_ge(self._semh(key), val)

    def _deps(self, eng, reads, writes):
        for r in reads:
            if r in self.lastw:
                self._wait(eng, self.lastw[r])
        for w in writes:
            if w in self.lastw:
                self._wait(eng, self.lastw[w])
            for k, v in self.readers.get(w, {}).items():
                self._wait(eng, (k, v))

    def _commit(self, tok, reads, writes):
        for r in reads:
            d = self.readers.setdefault(r, {})
            if d.get(tok[0], 0) < tok[1]:
                d[tok[0]] = tok[1]
        for w in writes:
            self.lastw[w] = tok
            self.readers[w] = {}

    def op(self, eng, fn, reads=(), writes=(), sig=True):
        self._deps(eng, reads, writes)
        ins = fn(self.engs[eng])
        self.nops += 1
        if sig:
            self.cnt[eng] += 1
            tok = (eng, self.cnt[eng])
            ins.then_inc(self.sems[eng], 1)
        else:
            tok = (eng, self.cnt[eng] + 1)
        self._commit(tok, reads, writes)
        return tok

    def dma(self, q, out, in_, reads=(), writes=()):
        self._deps(q, reads, writes)
        lo, hi = (0, NDMA_SP) if q == "sp" else (NDMA_SP, NDMA)
        i = lo + self.dnext.get(q, 0)
        self.dnext[q] = (self.dnext.get(q, 0) + 1) % (hi - lo)
        if self.dval[i] > 0:
            self._wait(q, (("d", i), self.dval[i]))
        ins = self.engs[q].dma_start(out=out, in_=in_)
        self.dval[i] += 16
        tok = (("d", i), self.dval[i])
        ins.then_inc(self.dsem[i], 16)
        self.nops += 1
        self._commit(tok, reads, writes)
        return tok

    def wait_all(self, eng, res):
        for r in res:
            if r in self.lastw:
                self._wait(eng, self.lastw[r])


class Builder:
    def __init__(self, layers):
        self.layers = list(layers)
        nc = self.nc = bass.Bass("TRN2", target_bir_lowering=False)
        self.es = ExitStack()
        es = self.es

        def dt(name, shape, kind="ExternalInput"):
            return nc.dram_tensor(name, shape, F32, kind=kind).ap()

        self.xT = dt("xT", [D, T])
        self.memT = dt("memT", [D, NMEM])
        self.consts_d = dt("consts", [128, 384])
        self.params_d = dt("params", [128, 4 * NPAR])
        self.win_d = {0: dt("win_g", [2, 128, KC * 2576]), 1: dt("win_c", [2, 128, KC * 2560])}
        self.wout_d = {0: dt("wout_g", [2, 128, 10 * 1024]), 1: dt("wout_c", [2, 128, 8 * 1024])}
        self.wa2_d = dt("wa2", [2, 17, 384])
        self.wkv_d = dt("wkv", [4, 128, KC * 512])
        self.wup_d = dt("wup", [4, 22, 128, KC * 256])
        self.wdn_d = dt("wdn", [4, 8, 128, 22 * 128])
        self.yT = dt("yT", [D, T], kind="ExternalOutput")

        def sb(name, shape, dtype):
            return es.enter_context(nc.sbuf_tensor(name, shape, dtype))

        self.x32 = sb("x32", [128, KC * T], F32)
        self.U = sb("U", [128, USZ], BF16)
        self.W2 = sb("W2", [128, 10240], BF16)
        self.MIb = sb("MIb", [128, KC * T], BF16)
        self.MIf = sb("MIf", [128, 4224], F32)
        self.consts = sb("consts_s", [128, 384], F32)
        self.params = sb("params_s", [128, 4 * NPAR], F32)
        self.ones = sb("ones", [128, 128], BF16)
        self.memTb = sb("memTb", [128, KC * NMEM], BF16)
        self.memK = sb("memK", [128, 2 * NMEM], BF16)
        self.memV = sb("memV", [128, 2 * 256], BF16)
        self.wa2 = sb("wa2s", [17, 384], F32)
        self.halo = sb("halo", [128, 16], F32)
        self.scr = sb("scr", [128, 8], F32)
        self.rcb = sb("rcb", [128, 512], F32)
        self.tmpA = sb("tmpA", [128, 512], F32)
        self.tmpB = sb("tmpB", [128, 512], F32)
        self.tmpC = sb("tmpC", [128, 512], F32)
        self.tmpD = sb("tmpD", [128, 512], F32)
        self.negmean = sb("negmean", [128, TTM], F32)
        self.rstd = sb("rstd", [128, TTM], F32)
        self.nmr = sb("nmr", [128, TTM], F32)
        self.lnz = sb("lnz", [128, 2 * KC * TTM], BF16)
        self.bg = []
        self.ps = [es.enter_context(nc.psum_tensor("ps%d" % i, [128, 512], F32)) for i in range(8)]

        self.t = Trk(nc, es)
        self.bank_ctr = {}

        tt = TTM
        o = 0

        def carve(n):
            nonlocal o
            a = o
            o += n
            return a
        a0 = carve(KC * tt)
        self.o_xbt = [a0, a0]
        self.o_qd = carve(4 * tt)
        self.o_ki = carve(4 * tt)
        self.o_kend = carve((tt // 128) * 384)
        self.o_vtok = carve((tt // 128) * 768)
        self.o_att = carve(4 * 128)
        self.o_stb = carve(4 * 2 * 192)
        self.o_memq = carve(2 * tt)
        self.o_expp = carve(2 * 2 * tt)
        self.o_mix = carve(10 * tt)
        self.o_sq = carve(2 * tt)
        self.o_wkv = self.o_qd
        assert self.o_qd + 4096 <= self.o_stb
        assert o <= KC * T, o
        o = 0
        self.f_xg = carve(tt)
        self.f_ltok = carve(384)
        self.f_et = carve(tt)
        self.f_eend = carve(384)
        self.f_state = carve(4 * 192)
        self.f_decay = carve(16)
        self.f_rrms = carve(tt)
        self.f_hs = [carve(tt), carve(tt)]
        self.f_ch = [carve(tt + 2), carve(tt + 2)]
        assert o <= 4224, o
        self.lay = {0: {k: getattr(self, k) for k in ("o_xbt", "o_memq", "o_expp", "o_mix", "o_wkv", "f_hs", "f_ch")}}
        c512 = 512
        self.lay[1] = {"o_xbt": [0, 0], "o_memq": KC * c512, "o_expp": KC * c512 + 2 * c512,
                       "o_mix": KC * c512 + 2 * c512 + 4 * c512, "o_wkv": KC * c512 + 2 * c512 + 4 * c512 + 8 * c512,
                       "f_hs": [0, c512], "f_ch": [2 * c512, 3 * c512 + 2]}
        assert self.lay[1]["o_wkv"] + 4096 <= KC * T and 4 * c512 + 4 <= 4224
        self.f_ug = 0
        self.f_uv = 2 + T + 62
        assert self.f_uv + 2 + T <= 4224

    def bank(self, lo=0, hi=8):
        k = (lo, hi)
        c = self.bank_ctr.get(k, 0)
        self.bank_ctr[k] = c + 1
        return lo + (c % (hi - lo))

    def mm_group(self, out_ap, out_res, pairs, reads):
        n = len(pairs)
        for i, (l, r) in enumerate(pairs):
            self.t.op("pe", lambda e, l=l, r=r, i=i: e.matmul(out_ap, l, r, start=(i == 0), stop=(i == n - 1)),
                      reads=reads, writes=[out_res], sig=(i == n - 1))

    def par(self, l, col, rows=128):
        c = l * NPAR + col
        return self.params[0:rows, c:c + 1]

    def barrier(self, toks):
        self.t.op("dve", lambda e: e.memset(self.scr[:, 0:1], 0.0), reads=[], writes=list(toks) + ["scr"])

    def prologue(self):
        t = self.t
        t.dma("sp", self.consts[:], self.consts_d, writes=["consts"])
        t.dma("sp", self.params[:], self.params_d, writes=["params"])
        xv = self.xT.rearrange("(kc p) t -> p kc t", p=128)
        x3 = self.x32[:].rearrange("p (kc t) -> p kc t", kc=KC)
        t.dma("sp", x3[:, :, 0:TTM], xv[:, :, 0:TTM], writes=[("x32", 0)])
        self.x_rest = [(x3[:, :, q * TTM:(q + 1) * TTM], xv[:, :, q * TTM:(q + 1) * TTM], q) for q in range(1, T // TTM)]
        mv = self.memT.rearrange("(kc p) m -> p kc m", p=128)
        t.dma("pool", self.memTb[:].rearrange("p (kc m) -> p kc m", kc=KC), mv, writes=["memTb"])
        t.op("dve", lambda e: e.memset(self.ones[:], 1.0), writes=["ones"])
        t.op("dve", lambda e: e.memset(self.scr[:], 0.0), writes=["scr", "scr1"])

    def ublk(self, lo, hi):
        return [("U", q) for q in range(lo // 1024, (hi - 1) // 1024 + 1)]

    def load_win(self, l, pieces):
        t = self.t
        typ = l % 2
        j = l // 2
        nin = NIN[typ]
        step = KC * nin // 4
        for pc in pieces:
            t.dma("pool", self.U[:, pc * step:(pc + 1) * step], self.win_d[typ][j, :, pc * step:(pc + 1) * step],
                  reads=[], writes=self.ublk(pc * step, (pc + 1) * step))

    def load_wout(self, l):
        t = self.t
        typ = l % 2
        j = l // 2
        nko = NKO[typ]
        half = nko * 1024 // 2
        for pc in range(2):
            t.dma("pool", self.W2[:, pc * half:(pc + 1) * half], self.wout_d[typ][j, :, pc * half:(pc + 1) * half],
                  reads=[], writes=(["W2_own"] if pc == 0 else []) + [("wout", pc)])
        if typ == 0:
            t.dma("sp", self.wa2[:], self.wa2_d[j], writes=["wa2"])

    def win(self, typ, kc, c0, n):
        nin = NIN[typ]
        a = kc * nin + c0
        return self.U[:, a:a + n]

    def win_res(self, typ, c0=None, n=None):
        nin = NIN[typ]
        r = []
        for kc in range(KC):
            for x in self.ublk(kc * nin + c0, kc * nin + c0 + n):
                if x not in r:
                    r.append(x)
        return r

    def wout(self, typ, i, o, rows):
        a = i * 1024 + o * 128
        return self.W2[0:rows, a:a + 128]

    def mem_kv(self, l):
        t = self.t
        wk = self.MIb[:, self.o_wkv:self.o_wkv + 4096]
        t.dma("pool", wk, self.wkv_d[l], reads=["MI_own"], writes=["wkv"])
        for hp in range(2):
            b = self.bank()
            pairs = [(self.MIb[:, self.o_wkv + kc * 512 + hp * 128: self.o_wkv + kc * 512 + hp * 128 + 128],
                      self.memTb[:, kc * NMEM:(kc + 1) * NMEM]) for kc in range(KC)]
            self.mm_group(self.ps[b][:, 0:NMEM], ("ps", b), pairs, ["wkv", "memTb", "MI_own"])
            t.op("act", lambda e, b=b, hp=hp: e.copy(self.memK[:, hp * NMEM:(hp + 1) * NMEM], self.ps[b][:, 0:NMEM]),
                 reads=[("ps", b)], writes=["memK"])
        for mc in range(2):
            b = self.bank()
            pairs = [(self.memTb[:, kc * NMEM + mc * 128: kc * NMEM + mc * 128 + 128],
                      self.MIb[:, self.o_wkv + kc * 512 + 256: self.o_wkv + kc * 512 + 512]) for kc in range(KC)]
            self.mm_group(self.ps[b][:, 0:256], ("ps", b), pairs, ["wkv", "memTb", "MI_own"])
            t.op("act", lambda e, b=b, mc=mc: e.copy(self.memV[:, mc * 256:(mc + 1) * 256], self.ps[b][:, 0:256]),
                 reads=[("ps", b)], writes=["memV"])

    def inproj_fm(self, typ, c0, m, tt, out_ap, out_res):
        xo = self.o_xbt[self.xb]
        pairs = [(self.win(typ, kc, c0, m), self.MIb[:, xo + kc * tt: xo + (kc + 1) * tt])
                 for kc in range(KC)]
        self.mm_group(out_ap, out_res, pairs, self.win_res(typ, c0, m) + ["xbt", "MI_own"])
        self.tick()

    def inproj_tm(self, typ, c0, n, tt, c, out_ap, out_res):
        xo = self.o_xbt[self.xb]
        pairs = [(self.MIb[:, xo + kc * tt + c * 128: xo + kc * tt + (c + 1) * 128],
                  self.win(typ, kc, c0, n)) for kc in range(KC)]
        self.mm_group(out_ap, out_res, pairs, self.win_res(typ, c0, n) + ["xbt", "MI_own"])
        self.tick()

    def cast_tile(self, ti, tt):
        t = self.t
        t0 = ti * tt
        self.xb = ti % 2
        xo = self.o_xbt[self.xb]
        for kc in range(KC):
            src = self.x32[:, kc * T + t0: kc * T + t0 + tt]
            dst = self.MIb[:, xo + kc * tt: xo + (kc + 1) * tt]
            eng = "pool" if kc % 2 == 0 else "act"
            if eng == "act":
                t.op("act", lambda e, s=src, d=dst: e.copy(d, s), reads=self.xr(ti, tt) + ["MI_own"], writes=["xbt"])
            else:
                t.op("pool", lambda e, s=src, d=dst: e.tensor_copy(d, s), reads=self.xr(ti, tt) + ["MI_own"], writes=["xbt"])

    def xr(self, ti, tt):
        return [("x32", (ti * tt) // TTM + i) for i in range(tt // TTM)]

    def tick(self, k=1):
        for _ in range(k):
            if not self.bg:
                return
            try:
                next(self.bg[0])
            except StopIteration:
                self.bg.pop(0)

    def drain(self, upto=None):
        while self.bg:
            if upto is not None and upto not in self.bg:
                return
            try:
                next(self.bg[0])
            except StopIteration:
                self.bg.pop(0)

    def ln_task(self, l, gcol, bcol, ti):
        t = self.t
        n = TTM
        t0 = ti * n
        xres = [("x32", ti)]
        for o in range(KC):
            z = self.x32[:, o * T + t0: o * T + t0 + n]
            zb = self.lnz[:, o * n:(o + 1) * n]
            zq = self.lnz[:, (KC + o) * n:(KC + o + 1) * n]
            t.op("dve", lambda e, z=z, zb=zb: e.tensor_copy(zb, z), reads=xres, writes=["lnzb"])
            t.op("act", lambda e, z=z, zq=zq: e.activation(out=zq, in_=z, func=AF.Square), reads=xres, writes=["lnzq"])
            yield
        for _ in range(3):
            yield
        bs = 7
        pm = [(self.ones[:], self.lnz[:, o * n:(o + 1) * n]) for o in range(KC)]
        pq = [(self.ones[:], self.lnz[:, (KC + o) * n:(KC + o + 1) * n]) for o in range(KC)]
        self.mm_group(self.ps[bs][:, 0:n], ("ps", bs), pm, ["ones", "lnzb"])
        yield
        self.mm_group(self.ps[bs][:, n:2 * n], ("ps", bs), pq, ["ones", "lnzq"])
        yield
        yield
        nm = self.negmean[:, 0:n]
        rs = self.rstd[:, 0:n]
        nr = self.nmr[:, 0:n]
        t.op("act", lambda e: e.mul(nm, self.ps[bs][:, 0:n], -1.0 / D), reads=[("ps", bs)], writes=["negmean"])
        yield
        t.op("act", lambda e: e.activation(out=nr, in_=nm, func=AF.Square), reads=["negmean"], writes=["nmr"])
        yield
        t.op("dve", lambda e: e.scalar_tensor_tensor(out=rs, in0=self.ps[bs][:, n:2 * n], scalar=1.0 / D, in1=nr,
                                                      op0=ALU.mult, op1=ALU.subtract),
             reads=[("ps", bs), "nmr"], writes=["rstd"])
        yield
        t.op("act", lambda e: e.activation(out=rs, in_=rs, func=AF.Ln, bias=EPS, scale=1.0), reads=["rstd"], writes=["rstd"])
        yield
        t.op("act", lambda e: e.activation(out=rs, in_=rs, func=AF.Exp, scale=-0.5), reads=["rstd"], writes=["rstd"])
        yield
        t.op("dve", lambda e: e.tensor_tensor(out=nr, in0=nm, in1=rs, op=ALU.mult), reads=["negmean", "rstd"], writes=["nmr"])
        yield
        for o in range(KC):
            z = self.x32[:, o * T + t0: o * T + t0 + n]
            g = self.par(l, gcol + o)
            bb = self.par(l, bcol + o)
            cr = ("x32c", ti, o)
            t.op("dve", lambda e, z=z: e.tensor_tensor(out=z, in0=z, in1=rs, op=ALU.mult), reads=xres + ["rstd"], writes=[cr])
            t.op("dve", lambda e, z=z: e.tensor_tensor(out=z, in0=z, in1=nr, op=ALU.add), reads=["nmr"], writes=[cr])
            yield
            t.op("act", lambda e, z=z, g=g, bb=bb: e.activation(out=z, in_=z, func=AF.Identity, bias=bb, scale=g),
                 reads=["params"], writes=[cr])
            yield
        t.op("act", lambda e: e.copy(self.scr[:, 1:2], self.scr[:, 2:3]), reads=[("x32c", ti, o) for o in range(KC)],
             writes=xres + ["scr1"])

    def outproj_residual(self, typ, ti, tt):
        t = self.t
        t0 = ti * tt
        nko = NKO[typ]
        for o in range(KC):
            b = self.bank(0, 7)
            pairs = []
            for i in range(nko):
                rows = 96 if (typ == 0 and i < 8) else 128
                pairs.append((self.wout(typ, i, o, rows), self.MIb[0:rows, self.o_mix + i * tt: self.o_mix + (i + 1) * tt]))
            self.mm_group(self.ps[b][:, 0:tt], ("ps", b), pairs, ["W2_own", ("wout", 0), ("wout", 1), "mixT", "MI_own"])
            z = self.x32[:, o * T + t0: o * T + t0 + tt]
            t.op("dve", lambda e, z=z, b=b: e.scalar_tensor_tensor(out=z, in0=z, scalar=ALPHA, in1=self.ps[b][:, 0:tt],
                                                                  op0=ALU.mult, op1=ALU.add),
                 reads=[("ps", b)] + self.xr(ti, tt), writes=self.xr(ti, tt))
            self.tick()

    def xattn_gen(self, typ, ti, tt, memq_c0, lo):
        t = self.t
        for hp in range(2):
            b = self.bank(lo, 7)
            self.inproj_fm(typ, memq_c0 + hp * 128, 128, tt, self.ps[b][:, 0:tt], ("ps", b))
            t.op("act", lambda e, b=b, hp=hp: e.copy(self.MIb[:, self.o_memq + hp * tt: self.o_memq + (hp + 1) * tt], self.ps[b][:, 0:tt]),
                 reads=[("ps", b), "MI_own"], writes=[("memq", hp)])
            yield

        def s_stage(h):
            hp, hh = h // 2, h % 2
            r0 = 64 * hh
            eb = h % 2
            for mc in range(2):
                b = self.bank(lo, 7)
                t.op("pe", lambda e, b=b, hp=hp, mc=mc, r0=r0: e.matmul(
                    self.ps[b][:, 0:tt],
                    self.memK[r0:r0 + 64, hp * NMEM + mc * 128: hp * NMEM + mc * 128 + 128],
                    self.MIb[r0:r0 + 64, self.o_memq + hp * tt: self.o_memq + (hp + 1) * tt],
                    start=True, stop=True),
                    reads=["memK", ("memq", hp), "MI_own"], writes=[("ps", b)])
                dst = self.MIb[:, self.o_expp + (eb * 2 + mc) * tt: self.o_expp + (eb * 2 + mc + 1) * tt]
                t.op("act", lambda e, b=b, dst=dst: e.activation(out=dst, in_=self.ps[b][:, 0:tt], func=AF.Exp, scale=0.125),
                     reads=[("ps", b), "MI_own"], writes=[("expp", eb, mc)])

        def pv_stage(h):
            hp, hh = h // 2, h % 2
            r0 = 64 * hh
            eb = h % 2
            bo = self.bank(lo, 7)
            bs = self.bank(lo, 7)
            pairs_o, pairs_s = [], []
            for mc in range(2):
                ep = self.MIb[:, self.o_expp + (eb * 2 + mc) * tt: self.o_expp + (eb * 2 + mc + 1) * tt]
                pairs_o.append((self.memV[:, mc * 256 + hp * 128: mc * 256 + hp * 128 + 128], ep))
                pairs_s.append((self.ones[:], ep))
            rd = ["memV", "ones", ("expp", eb, 0), ("expp", eb, 1), "MI_own"]
            self.mm_group(self.ps[bo][:, 0:tt], ("ps", bo), pairs_o, rd)
            self.mm_group(self.ps[bs][:, 0:tt], ("ps", bs), pairs_s, rd)
            rc = self.rcb[r0:r0 + 64, 0:tt]
            t.op("dve", lambda e, bs=bs, rc=rc, r0=r0: e.reciprocal(rc, self.ps[bs][r0:r0 + 64, 0:tt]),
                 reads=[("ps", bs)], writes=["rcb"])
            i = NKO[typ] - 2 + hp
            dst = self.MIb[r0:r0 + 64, self.o_mix + i * tt: self.o_mix + (i + 1) * tt]
            t.op("dve", lambda e, bo=bo, rc=rc, dst=dst, r0=r0: e.tensor_tensor(out=dst, in0=self.ps[bo][r0:r0 + 64, 0:tt], in1=rc, op=ALU.mult),
                 reads=[("ps", bo), "rcb", "MI_own"], writes=["mixT"])

        s_stage(0)
        yield
        for h in range(4):
            if h + 1 < 4:
                s_stage(h + 1)
                yield
            pv_stage(h)
            self.tick()
            yield

    def fill(self, k=1):
        for _ in range(k):
            if self.filler is None:
                return
            try:
                next(self.filler)
            except StopIteration:
                self.filler = None

    def fill_all(self):
        while self.filler is not None:
            self.fill()

    def conv_tile(self, l, ti, tt):
        t = self.t
        typ = 1
        PC = 208

        def stage1(ci):
            p = ci % 2
            hsn, chn = ("hs", p), ("ch", p)
            bh = self.bank(0, 7)
            self.inproj_fm(typ, 1536 + 128 * ci, 128, tt, self.ps[bh][:, 0:tt], ("ps", bh))
            hs = self.MIf[:, self.f_hs[p]:self.f_hs[p] + tt]
            t.op("act", lambda e, bh=bh, hs=hs: e.copy(hs, self.ps[bh][:, 0:tt]), reads=[("ps", bh), "MI_own"], writes=[hsn])
            bc = self.bank(0, 7)
            self.inproj_fm(typ, 768 + 128 * ci, 128, tt, self.ps[bc][:, 0:tt], ("ps", bc))
            ch = self.MIf[:, self.f_ch[p]:self.f_ch[p] + tt + 2]
            t.op("act", lambda e, ci=ci, ch=ch: e.copy(ch[:, 0:2], self.halo[:, 2 * ci:2 * ci + 2]),
                 reads=[("halo", ci), "MI_own"], writes=[chn])
            t.op("dve", lambda e, bc=bc, hs=hs, ch=ch: e.tensor_tensor(out=ch[:, 2:tt + 2], in0=self.ps[bc][:, 0:tt], in1=hs, op=ALU.mult),
                 reads=[("ps", bc), hsn, "MI_own"], writes=[chn])

        def stage2(ci):
            p = ci % 2
            chn = ("ch", p)
            an = "tmpA" if p == 0 else "tmpB"
            ch = self.MIf[:, self.f_ch[p]:self.f_ch[p] + tt + 2]
            t.op("act", lambda e, ci=ci, ch=ch: e.copy(self.halo[:, 2 * ci:2 * ci + 2], ch[:, tt:tt + 2]),
                 reads=[chn], writes=[("halo", ci)])
            a = (self.tmpA if p == 0 else self.tmpB)[:, 0:tt]
            w0, w1, w2 = (self.par(l, PC + ci * 3 + k) for k in range(3))
            t.op("act", lambda e, a=a, ch=ch, w2=w2: e.mul(a, ch[:, 2:tt + 2], w2), reads=[chn, "params"], writes=[an])
            t.op("dve", lambda e, a=a, ch=ch, w1=w1: e.scalar_tensor_tensor(out=a, in0=ch[:, 1:tt + 1], scalar=w1, in1=a, op0=ALU.mult, op1=ALU.add),
                 reads=[chn, "params", an], writes=[an])
            t.op("dve", lambda e, a=a, ch=ch, w0=w0: e.scalar_tensor_tensor(out=a, in0=ch[:, 0:tt], scalar=w0, in1=a, op0=ALU.mult, op1=ALU.add),
                 reads=[chn, "params", an], writes=[an])
            bb = self.bank(0, 7)
            self.inproj_fm(typ, 128 * ci, 128, tt, self.ps[bb][:, 0:tt], ("ps", bb))
            dst = self.MIb[:, self.o_mix + ci * tt: self.o_mix + (ci + 1) * tt]
            t.op("dve", lambda e, bb=bb, a=a, dst=dst: e.tensor_tensor(out=dst, in0=self.ps[bb][:, 0:tt], in1=a, op=ALU.mult),
                 reads=[("ps", bb), an, "MI_own"], writes=["mixT"])
            self.tick(5)

        stage1(0)
        for ci in range(6):
            if ci + 1 < 6:
                stage1(ci + 1)
            stage2(ci)

    def gla_tile(self, l, ti, tt):
        t = self.t
        typ = 0
        nch = tt // 128
        triF = self.consts[:, 0:128]
        triR = self.consts[:, 128:256]
        maskT = self.consts[:, 256:384]
        PH = 226

        def gps(h):
            return self.ps[h // 2][0:96, (h % 2) * tt:(h % 2 + 1) * tt]
        b = self.bank(2, 4)
        self.inproj_fm(typ, 2304, 16, tt, self.ps[b][0:16, 0:tt], ("ps", b))
        xg = self.MIf[0:17, self.f_xg:self.f_xg + tt]
        t.op("act", lambda e, b=b: e.copy(self.MIf[0:16, self.f_xg:self.f_xg + tt], self.ps[b][0:16, 0:tt]),
             reads=[("ps", b), "MI_own"], writes=["xg"])
        ltok = self.MIf[:, self.f_ltok:self.f_ltok + 384]
        eend = self.MIf[:, self.f_eend:self.f_eend + 384]
        for c in range(nch):
            b = self.bank(2, 4)
            t.op("pe", lambda e, b=b, c=c: e.matmul(self.ps[b][:, 0:384], xg[:, c * 128:(c + 1) * 128], self.wa2[:, :], start=True, stop=True),
                 reads=["xg", "wa2", "MI_own"], writes=[("ps", b)])
            t.op("act", lambda e, b=b: e.activation(out=ltok, in_=self.ps[b][:, 0:384], func=AF.Exp, scale=-1.0),
                 reads=[("ps", b), "MI_own"], writes=["ltok"])
            t.op("act", lambda e: e.activation(out=ltok, in_=ltok, func=AF.Ln, bias=1.0, scale=1.0), reads=["ltok"], writes=["ltok"])
            for half in range(2):
                b4 = self.bank(4, 7)
                self.inproj_tm(typ, 768 + 384 * half, 384, tt, c, self.ps[b4][:, 0:384], ("ps", b4))
                vd = self.MIb[:, self.o_vtok + c * 768 + half * 384: self.o_vtok + c * 768 + (half + 1) * 384]
                t.op("act", lambda e, b4=b4, vd=vd: e.copy(vd, self.ps[b4][:, 0:384]),
                     reads=[("ps", b4), "MI_own"], writes=[("vtok", c)])
            for h in range(4):
                t.op("pe", lambda e, h=h, c=c: e.matmul(gps(h)[:, c * 128:(c + 1) * 128], ltok[:, 96 * h:96 * h + 96], triF, start=True, stop=True),
                     reads=["ltok", "consts", "MI_own"], writes=[("ps", h // 2)])
            b2 = self.bank(2, 4)
            t.op("pe", lambda e, b2=b2: e.matmul(self.ps[b2][:, 0:384], triR, ltok, start=True, stop=True),
                 reads=["ltok", "consts", "MI_own"], writes=[("ps", b2)])
            t.op("act", lambda e, b2=b2: e.activation(out=eend, in_=self.ps[b2][:, 0:384], func=AF.Exp),
                 reads=[("ps", b2), "MI_own"], writes=["eend"])
            b3 = self.bank(4, 7)
            self.inproj_tm(typ, 384, 384, tt, c, self.ps[b3][:, 0:384], ("ps", b3))
            kd = self.MIb[:, self.o_kend + c * 384: self.o_kend + (c + 1) * 384]
            t.op("dve", lambda e, b3=b3, kd=kd: e.tensor_tensor(out=kd, in0=self.ps[b3][:, 0:384], in1=eend, op=ALU.mult),
                 reads=[("ps", b3), "eend", "MI_own"], writes=[("kend", c)])
        et = self.MIf[0:96, self.f_et:self.f_et + tt]
        for h in range(4):
            bq = self.bank(4, 7)
            self.inproj_fm(typ, 96 * h, 96, tt, self.ps[bq][0:96, 0:tt], ("ps", bq))
            bk = self.bank(4, 7)
            self.inproj_fm(typ, 384 + 96 * h, 96, tt, self.ps[bk][0:96, 0:tt], ("ps", bk))
            t.op("act", lambda e, h=h: e.activation(out=et, in_=gps(h), func=AF.Exp),
                 reads=[("ps", h // 2), "MI_own"], writes=["et"])
            dec = self.MIf[0:96, self.f_decay + h * 4: self.f_decay + h * 4 + nch]
            t.op("dve", lambda e, dec=dec: e.tensor_copy(dec, et[:, 127:tt:128]), reads=["et"], writes=[("decay", h)])
            qd = self.MIb[0:96, self.o_qd + h * tt: self.o_qd + (h + 1) * tt]
            t.op("dve", lambda e, bq=bq, qd=qd: e.scalar_tensor_tensor(out=qd, in0=self.ps[bq][0:96, 0:tt], scalar=QSCALE, in1=et,
                                                                       op0=ALU.mult, op1=ALU.mult),
                 reads=[("ps", bq), "et", "MI_own"], writes=[("qd", h)])
            einv = self.tmpD[0:96, 0:tt]
            t.op("dve", lambda e, einv=einv: e.reciprocal(einv, et), reads=["et"], writes=["tmpD"])
            ki = self.MIb[0:96, self.o_ki + h * tt: self.o_ki + (h + 1) * tt]
            t.op("dve", lambda e, bk=bk, ki=ki, einv=einv: e.tensor_tensor(out=ki, in0=self.ps[bk][0:96, 0:tt], in1=einv, op=ALU.mult),
                 reads=[("ps", bk), "tmpD", "MI_own"], writes=[("ki", h)])
        self.filler = self.xattn_gen(typ, ti, tt, 2320, 2)
        tmps = [(self.tmpA, "tmpA"), (self.tmpB, "tmpB"), (self.tmpC, "tmpC"), (self.tmpD, "tmpD")]
        for hpair in range(2):
            heads = (2 * hpair, 2 * hpair + 1)
            srs = {}
            for h in heads:
                for vt in range(2):
                    i8 = 2 * h + vt
                    br = self.bank(4, 7)
                    self.inproj_fm(typ, 1536 + 96 * i8, 96, tt, self.ps[br][0:96, 0:tt], ("ps", br))
                    tb, tn = tmps[(h % 2) * 2 + vt]
                    sr = tb[0:96, 0:tt]
                    t.op("act", lambda e, br=br, sr=sr: e.activation(out=sr, in_=self.ps[br][0:96, 0:tt], func=AF.Silu),
                         reads=[("ps", br)], writes=[tn])
                    srs[(h, vt)] = (sr, tn)
            self.fill()
            for c in range(nch):
                gchunk = ti * nch + c
                cur = gchunk % 2
                nxt = 1 - cur
                cs = slice(c * 128, (c + 1) * 128)
                bas, bkvs = {}, {}
                for h in heads:
                    qd = self.MIb[0:96, self.o_qd + h * tt: self.o_qd + (h + 1) * tt]
                    ki = self.MIb[0:96, self.o_ki + h * tt: self.o_ki + (h + 1) * tt]
                    ba = self.bank(2, 4)
                    t.op("pe", lambda e, ba=ba, cs=cs, ki=ki, qd=qd: e.matmul(self.ps[ba][:, 0:128], ki[:, cs], qd[:, cs], start=True, stop=True),
                         reads=[("ki", h), ("qd", h), "MI_own"], writes=[("ps", ba)])
                    bkv = self.bank(4, 7)
                    t.op("pe", lambda e, bkv=bkv, c=c, h=h: e.matmul(
                        self.ps[bkv][0:96, 0:192], self.MIb[:, self.o_kend + c * 384 + 96 * h: self.o_kend + c * 384 + 96 * h + 96],
                        self.MIb[:, self.o_vtok + c * 768 + h * 192: self.o_vtok + c * 768 + (h + 1) * 192], start=True, stop=True),
                        reads=[("kend", c), ("vtok", c), "MI_own"], writes=[("ps", bkv)])
                    bas[h], bkvs[h] = ba, bkv
                for h in heads:
                    qd = self.MIb[0:96, self.o_qd + h * tt: self.o_qd + (h + 1) * tt]
                    pb = h % 2
                    st32 = self.MIf[0:96, self.f_state + h * 192: self.f_state + (h + 1) * 192]
                    stb_cur = self.MIb[0:96, self.o_stb + (h * 2 + cur) * 192: self.o_stb + (h * 2 + cur + 1) * 192]
                    stb_nxt = self.MIb[0:96, self.o_stb + (h * 2 + nxt) * 192: self.o_stb + (h * 2 + nxt + 1) * 192]
                    ba, bkv = bas[h], bkvs[h]
                    ab = (h % 2) * 2 + c % 2
                    att = self.MIb[:, self.o_att + ab * 128: self.o_att + (ab + 1) * 128]
                    t.op("dve", lambda e, ba=ba, att=att: e.tensor_tensor(out=att, in0=self.ps[ba][:, 0:128], in1=maskT, op=ALU.mult),
                         reads=[("ps", ba), "consts", "MI_own"], writes=[("att", ab)])
                    dcol = self.f_decay + h * 4 + c
                    t.op("dve", lambda e, bkv=bkv, st32=st32, dcol=dcol: e.scalar_tensor_tensor(
                        out=st32, in0=st32, scalar=self.MIf[0:96, dcol:dcol + 1], in1=self.ps[bkv][0:96, 0:192], op0=ALU.mult, op1=ALU.add),
                        reads=[("ps", bkv), ("decay", h), ("state", h), "MI_own"], writes=[("state", h)])
                    for vt in range(2):
                        vcol = self.o_vtok + c * 768 + h * 192 + vt * 96
                        osl = slice(vt * tt + c * 128, vt * tt + (c + 1) * 128)
                        t.op("pe", lambda e, vcol=vcol, att=att, osl=osl, pb=pb: e.matmul(
                            self.ps[pb][0:96, osl], self.MIb[:, vcol:vcol + 96], att, start=True, stop=False),
                            reads=[("vtok", c), ("att", ab), "MI_own"], writes=[("ps", pb)], sig=False)
                        t.op("pe", lambda e, vt=vt, cs=cs, osl=osl, stb_cur=stb_cur, qd=qd, pb=pb: e.matmul(
                            self.ps[pb][0:96, osl], stb_cur[:, vt * 96:(vt + 1) * 96], qd[:, cs], start=False, stop=True),
                            reads=[("stb", h, cur), ("qd", h), "MI_own"], writes=[("ps", pb)])
                    t.op("act", lambda e, st32=st32, stb_nxt=stb_nxt: e.copy(stb_nxt, st32),
                         reads=[("state", h), "MI_own"], writes=[("stb", h, nxt)])
                self.tick()
                self.fill()
            for h in heads:
                pb = h % 2
                bss = self.bank(2, 4)
                sq = self.MIb[0:96, self.o_sq: self.o_sq + 2 * tt]
                t.op("act", lambda e, sq=sq, pb=pb: e.activation(out=sq, in_=self.ps[pb][0:96, 0:2 * tt], func=AF.Square),
                     reads=[("ps", pb), "MI_own"], writes=["sq"])
                for vt in range(2):
                    t.op("pe", lambda e, vt=vt, bss=bss, sq=sq: e.matmul(self.ps[bss][0:96, 0:tt], self.ones[0:96, 0:96], sq[:, vt * tt:(vt + 1) * tt], start=(vt == 0), stop=(vt == 1)),
                         reads=["sq", "ones", "MI_own"], writes=[("ps", bss)], sig=(vt == 1))
                rr = self.MIf[0:96, self.f_rrms:self.f_rrms + tt]
                t.op("act", lambda e, bss=bss, rr=rr: e.activation(out=rr, in_=self.ps[bss][0:96, 0:tt], func=AF.Ln, bias=EPS, scale=1.0 / 192.0),
                     reads=[("ps", bss), "MI_own"], writes=["rrms"])
                t.op("act", lambda e, rr=rr: e.activation(out=rr, in_=rr, func=AF.Exp, scale=-0.5), reads=["rrms"], writes=["rrms"])
                self.fill()
                for vt in range(2):
                    i8 = 2 * h + vt
                    sr, srn = srs[(h, vt)]
                    hg = self.par(l, PH + i8, rows=96)
                    t.op("dve", lambda e, sr=sr, hg=hg, rr=rr: e.scalar_tensor_tensor(out=sr, in0=sr, scalar=hg, in1=rr, op0=ALU.mult, op1=ALU.mult),
                         reads=[srn, "rrms", "params"], writes=[srn])
                    dst = self.MIb[0:96, self.o_mix + i8 * tt: self.o_mix + (i8 + 1) * tt]
                    t.op("dve", lambda e, vt=vt, sr=sr, dst=dst, pb=pb: e.tensor_tensor(out=dst, in0=self.ps[pb][0:96, vt * tt:(vt + 1) * tt], in1=sr, op=ALU.mult),
                         reads=[("ps", pb), srn, "MI_own"], writes=["mixT"])
                self.tick()

    def mixer(self, l):
        t = self.t
        typ = l % 2
        tt = TTM if typ == 0 else 512
        for k, v in self.lay[typ].items():
            setattr(self, k, v)
        if getattr(self, "memkv_done", None) != l:
            self.mem_kv(l)
        if getattr(self, "wout_pending", None) == l:
            self.wout_pending = None
            self.load_wout(l)
        if typ == 0:
            t.op("dve", lambda e: e.memset(self.MIf[0:96, self.f_state:self.f_state + 768], 0.0),
                 reads=["MI_own"], writes=[("state", h) for h in range(4)])
            t.op("dve", lambda e: e.memset(self.MIb[0:96, self.o_stb:self.o_stb + 1536], 0.0),
                 reads=["MI_own"], writes=[("stb", h, k) for h in range(4) for k in range(2)])
            t.op("dve", lambda e: e.memset(self.MIf[0:17, self.f_xg:self.f_xg + tt], 1.0), reads=["MI_own"], writes=["xg"])
        else:
            t.op("dve", lambda e: e.memset(self.halo[:], 0.0), writes=[("halo", ci) for ci in range(6)])
        for ti in range(T // tt):
            if self.ln2_tasks:
                self.drain(upto=self.ln2_tasks[(ti + 1) * tt // TTM - 1])
            self.cast_tile(ti, tt)
            if typ == 0:
                self.gla_tile(l, ti, tt)
            else:
                self.filler = self.xattn_gen(typ, ti, tt, 2304, 0)
                self.conv_tile(l, ti, tt)
            self.fill_all()
            self.outproj_residual(typ, ti, tt)
            for i in range(tt // TTM):
                self.bg.append(self.ln_task(l, 0, 8, ti * tt // TTM + i))
        self.drain()

    def load_wup(self, l, j, buf):
        self.t.dma("pool", self.W2[:, buf * 2048:(buf + 1) * 2048], self.wup_d[l, j],
                   reads=[], writes=[("wupb", buf)] + (["W2_own"] if self.first_ffn_dma else []))
        self.first_ffn_dma = False

    def ffn(self, l, next_l):
        t = self.t
        tt = TTF
        ntile = T // tt
        PW, PB = 32, 164
        self.first_ffn_dma = True
        nwb = 3
        units = [(gi, jj) for gi, (s0, n) in enumerate(GROUPS) for jj in range(n)]
        for k in range(nwb - 1):
            gi, jj = units[k]
            self.load_wup(l, GROUPS[gi][0] + jj, k % nwb)
        self.barrier(["MI_own"])
        xr = [("x32", i) for i in range(T // TTM)]
        k = 0
        for hf in range(2):
            for kc in range(KC):
                src = self.x32[:, kc * T + hf * 1024: kc * T + (hf + 1) * 1024]
                dst = self.MIb[:, kc * T + hf * 1024: kc * T + (hf + 1) * 1024]
                xrh = [("x32", i) for i in range(hf * 1024 // TTM, (hf + 1) * 1024 // TTM)]
                if k % 2 == 0:
                    t.op("act", lambda e, s=src, d=dst: e.copy(d, s), reads=xrh + ["MI_own"], writes=[("xbf", kc, hf)])
                else:
                    t.op("dve", lambda e, s=src, d=dst: e.tensor_copy(d, s), reads=xrh + ["MI_own"], writes=[("xbf", kc, hf)])
                k += 1
        ug = self.MIf[:, self.f_ug:self.f_ug + 2 + T]
        uv = self.MIf[:, self.f_uv:self.f_uv + 2 + T]
        t.op("dve", lambda e: e.memset(ug[:, 0:2], 0.0), reads=["MI_own"], writes=[("ug", -1)])
        t.op("dve", lambda e: e.memset(uv[:, 0:2], 0.0), reads=["MI_own"], writes=[("uv", -1)])
        agb = [(self.tmpA, "tmpA"), (self.tmpC, "tmpC")]
        avb = [(self.tmpB, "tmpB"), (self.tmpD, "tmpD")]
        pend = []
        ucount = 0
        WDN0 = 6144
        last_gi = len(GROUPS) - 1

        def stage_b(item):
            jj, t0, p = item
            ag, agn = agb[p]
            av, avn = avb[p]
            t.op("act", lambda e, ag=ag: e.activation(out=ag[:, 0:tt], in_=ag[:, 0:tt], func=AF.Silu), reads=[agn], writes=[agn])
            hdst = self.U[:, jj * T + t0: jj * T + t0 + tt]
            t.op("pool", lambda e, ag=ag, av=av, hdst=hdst: e.tensor_tensor(out=hdst, in0=ag[:, 0:tt], in1=av[:, 0:tt], op=ALU.mult),
                 reads=[agn, avn], writes=self.ublk(jj * T + t0, jj * T + t0 + tt))

        def wdn_names(slot_off, n):
            return [("wdn", q) for q in range(slot_off // 1024, (slot_off + n * 128 - 1) // 1024 + 1)]

        def load_wdn(gi, o, slot_off, n):
            s0 = GROUPS[gi][0]
            t.dma("pool", self.W2[:, WDN0 + slot_off: WDN0 + slot_off + n * 128], self.wdn_d[l, o, :, s0 * 128:(s0 + n) * 128],
                  reads=[], writes=wdn_names(slot_off, n))

        def wdn_names(slot_off, n):
            return [("wdn", q) for q in range(slot_off // 1024, (slot_off + n * 128 - 1) // 1024 + 1)]

        for ui, (gi, jj) in enumerate(units):
            s0, gn = GROUPS[gi]
            j = s0 + jj
            buf = ui % nwb
            if ui + nwb - 1 < len(units):
                g2, jj2 = units[ui + nwb - 1]
                self.load_wup(l, GROUPS[g2][0] + jj2, (ui + nwb - 1) % nwb)
            if jj == 0:
                if gi < last_gi:
                    load_wdn(gi, 0, 0, gn)
                    load_wdn(gi, 1, 2048, gn)
                else:
                    for o in range(KC):
                        load_wdn(gi, o, o * gn * 128, gn)
            wg = [self.par(l, PW + j * 3 + k) for k in range(3)]
            wv = [self.par(l, PW + (22 + j) * 3 + k) for k in range(3)]
            bgp = self.par(l, PB + j)
            bvp = self.par(l, PB + 22 + j)
            for ti in range(ntile):
                t0 = ti * tt
                p = ucount % 2
                ucount += 1
                bg = self.bank(0, 7)
                bv = self.bank(0, 7)
                xbf_res = [("xbf", kc, t0 // 1024) for kc in range(KC)] + ["MI_own"]
                for which, bnk in ((0, bg), (1, bv)):
                    pairs = [(self.W2[:, buf * 2048 + kc * 256 + which * 128: buf * 2048 + kc * 256 + which * 128 + 128],
                              self.MIb[:, kc * T + t0: kc * T + t0 + tt]) for kc in range(KC)]
                    self.mm_group(self.ps[bnk][:, 0:tt], ("ps", bnk), pairs, [("wupb", buf), "W2_own"] + xbf_res)
                ag, agn = agb[p]
                av, avn = avb[p]
                paths = ((ug, "ug", bg, ag[:, 0:tt], agn, wg, bgp), (uv, "uv", bv, av[:, 0:tt], avn, wv, bvp))
                for (u, un, bnk, a, ares, w, bp) in paths:
                    t.op("act", lambda e, u=u, bnk=bnk: e.copy(u[:, 2 + t0:2 + t0 + tt], self.ps[bnk][:, 0:tt]),
                         reads=[("ps", bnk), "MI_own"], writes=[(un, ti)])
                if pend:
                    stage_b(pend.pop(0))
                for (u, un, bnk, a, ares, w, bp) in paths:
                    ur = [(un, ti), (un, ti - 1)]
                    t.op("act", lambda e, a=a, bnk=bnk, w=w, bp=bp: e.activation(out=a, in_=self.ps[bnk][:, 0:tt], func=AF.Identity, bias=bp, scale=w[2]),
                         reads=[("ps", bnk), "params"], writes=[ares])
                    t.op("dve", lambda e, a=a, u=u, w=w: e.scalar_tensor_tensor(out=a, in0=u[:, 1 + t0:1 + t0 + tt], scalar=w[1], in1=a, op0=ALU.mult, op1=ALU.add),
                         reads=ur + ["params", ares, "MI_own"], writes=[ares])
                    t.op("dve", lambda e, a=a, u=u, w=w: e.scalar_tensor_tensor(out=a, in0=u[:, t0:t0 + tt], scalar=w[0], in1=a, op0=ALU.mult, op1=ALU.add),
                         reads=ur + ["params", ares, "MI_own"], writes=[ares])
                pend.append((jj, t0, p))
            if jj == gn - 1:
                while pend:
                    stage_b(pend.pop(0))
                if gi < last_gi:
                    for o in range(KC):
                        so = (o % 2) * 2048
                        for ti in range(ntile):
                            t0 = ti * tt
                            b = self.bank(0, 7)
                            pairs = [(self.W2[:, WDN0 + so + q * 128: WDN0 + so + (q + 1) * 128], self.U[:, q * T + t0: q * T + t0 + tt]) for q in range(gn)]
                            hres = []
                            for q in range(gn):
                                hres += self.ublk(q * T + t0, q * T + t0 + tt)
                            self.mm_group(self.ps[b][:, 0:tt], ("ps", b), pairs, wdn_names(so, gn) + ["W2_own"] + hres)
                            z = self.x32[:, o * T + t0: o * T + t0 + tt]
                            xres = [("x32", t0 // TTM + i) for i in range(tt // TTM)]
                            if gi == 0:
                                t.op("dve", lambda e, z=z, b=b: e.scalar_tensor_tensor(out=z, in0=z, scalar=ALPHA, in1=self.ps[b][:, 0:tt], op0=ALU.mult, op1=ALU.add),
                                     reads=[("ps", b)] + xres, writes=xres)
                            else:
                                t.op("dve", lambda e, z=z, b=b: e.tensor_tensor(out=z, in0=z, in1=self.ps[b][:, 0:tt], op=ALU.add),
                                     reads=[("ps", b)] + xres, writes=xres)
                        if o + 2 < KC:
                            load_wdn(gi, o + 2, so, gn)
                else:
                    self.barrier(["MI_own"])
                    if next_l is not None:
                        self.o_wkv = self.lay[next_l % 2]["o_wkv"]
                        self.mem_kv(next_l)
                        self.memkv_done = next_l
                        self.load_win(next_l, [2, 3])
                    self.ln2_tasks = []
                    t2 = TTM
                    for ti in range(T // t2):
                        t0 = ti * t2
                        xres = [("x32", ti)]
                        hres = []
                        for q in range(gn):
                            hres += self.ublk(q * T + t0, q * T + t0 + t2)
                        for o in range(KC):
                            so = o * gn * 128
                            b = self.bank(0, 7)
                            pairs = [(self.W2[:, WDN0 + so + q * 128: WDN0 + so + (q + 1) * 128], self.U[:, q * T + t0: q * T + t0 + t2]) for q in range(gn)]
                            self.mm_group(self.ps[b][:, 0:t2], ("ps", b), pairs, wdn_names(so, gn) + ["W2_own"] + hres)
                            z = self.x32[:, o * T + t0: o * T + t0 + t2]
                            t.op("dve", lambda e, z=z, b=b: e.tensor_tensor(out=z, in0=z, in1=self.ps[b][:, 0:t2], op=ALU.add),
                                 reads=[("ps", b)] + xres, writes=xres)
                            self.tick(5)
                        task = self.ln_task(l, 16, 24, ti)
                        self.ln2_tasks.append(task)
                        self.bg.append(task)
                    if next_l is not None:
                        self.load_win(next_l, [0, 1])
                        self.load_wout(next_l)

    def build(self):
        t = self.t
        gl = [l for l in self.layers if l % 2 == 0]
        self.first_gla = gl[0] if gl else -1
        self.ln2_tasks = []
        self.prologue()
        self.load_win(self.layers[0], [0, 1, 2, 3])
        for (dst, src, q) in self.x_rest:
            t.dma("sp", dst, src, reads=self.ublk(0, USZ), writes=[("x32", q)])
        self.wout_pending = self.layers[0]
        for li, l in enumerate(self.layers):
            nl = self.layers[li + 1] if li + 1 < len(self.layers) else None
            self.mixer(l)
            self.ffn(l, nl)
        self.drain()
        yv = self.yT.rearrange("(kc p) t -> p kc t", p=128)
        x3 = self.x32[:].rearrange("p (kc t) -> p kc t", kc=KC)
        for q in range(4):
            t.dma("sp", yv[:, :, q * 512:(q + 1) * 512], x3[:, :, q * 512:(q + 1) * 512],
                  reads=[("x32", q * 512 // TTM + i) for i in range(512 // TTM)], writes=[("y", q)])
        t.wait_all("sp", [("y", q) for q in range(4)])
        return self.nc


def _prep_weights(inp):
    f = np.float32
    out = {}
    tri = np.arange(128)
    c = np.zeros((128, 384), f)
    c[:, 0:128] = np.where(tri[:, None] <= tri[None, :], -1.0 / 16.0, 0.0)
    c[:, 128:256] = np.where(tri[:, None] > tri[None, :], -1.0 / 16.0, 0.0)
    c[:, 256:384] = np.where(tri[:, None] <= tri[None, :], 1.0, 0.0)
    out["consts"] = c
    par = np.zeros((128, 4 * NPAR), f)
    for l in range(4):
        b = l * NPAR
        j = l // 2
        for name, col in (("ln1_g", 0), ("ln1_b", 8), ("ln2_g", 16), ("ln2_b", 24)):
            par[:, b + col:b + col + 8] = np.asarray(inp[name][l]).reshape(8, 128).T
        cw = np.asarray(inp["ffn_conv_w"][l])
        par[:, b + 32:b + 32 + 132] = cw.reshape(3, 44, 128).transpose(2, 1, 0).reshape(128, 132)
        par[:, b + 164:b + 208] = np.asarray(inp["ffn_conv_b"][l]).reshape(44, 128).T
        if l % 2 == 1:
            mw = np.asarray(inp["conv_w"][j])
            par[:, b + 208:b + 226] = mw.reshape(3, 6, 128).transpose(2, 1, 0).reshape(128, 18)
        else:
            par[0:96, b + 226:b + 234] = np.asarray(inp["gla_head_g"][j]).reshape(8, 96).T
    out["params"] = par

    def kmajor(w):
        L, K, N = w.shape
        return np.ascontiguousarray(w.reshape(L, K // 128, 128, N).transpose(0, 2, 1, 3).reshape(L, 128, (K // 128) * N))
    out["win_g"] = kmajor(np.asarray(inp["gla_w_in"], f))
    out["win_c"] = kmajor(np.asarray(inp["conv_w_in"], f))
    out["wkv"] = kmajor(np.asarray(inp["w_mem_kv"], f))
    out["wout_c"] = kmajor(np.asarray(inp["conv_w_out"], f))
    gw = np.asarray(inp["gla_w_out"], f)
    wg = np.zeros((2, 128, 10, 1024), f)
    wg[:, 0:96, 0:8, :] = gw[:, 0:768, :].reshape(2, 8, 96, 1024).transpose(0, 2, 1, 3)
    wg[:, :, 8:10, :] = gw[:, 768:1024, :].reshape(2, 2, 128, 1024).transpose(0, 2, 1, 3)
    out["wout_g"] = wg.reshape(2, 128, 10 * 1024)
    out["wa2"] = np.concatenate([np.asarray(inp["gla_w_a2"], f), np.asarray(inp["gla_b_a"], f)[:, None, :]], axis=1)
    wu = np.asarray(inp["ffn_w_up"], f).reshape(4, 8, 128, 2, 22, 128)
    out["wup"] = np.ascontiguousarray(wu.transpose(0, 4, 2, 1, 3, 5).reshape(4, 22, 128, 8 * 256))
    wd = np.asarray(inp["ffn_w_down"], f).reshape(4, 22, 128, 8, 128)
    out["wdn"] = np.ascontiguousarray(wd.transpose(0, 3, 2, 1, 4).reshape(4, 8, 128, 22 * 128))
    return out


_CACHE = {}


def _get_nc(layers):
    key = tuple(layers)
    if key not in _CACHE:
        _CACHE[key] = Builder(layers).build()
    return _CACHE[key]


def _run(layers, xT_list, memT_list, w):
    nc = _get_nc(layers)
    in_maps = []
    for c in range(8):
        m = dict(w)
        m["xT"] = xT_list[c]
        m["memT"] = memT_list[c]
        in_maps.append(m)
    res = run_bass_kernel_spmd(nc, in_maps, core_ids=list(range(8)))
    return [np.asarray(r["yT"]) for r in res.results]


LAUNCH_GROUPS = [[0, 1, 2, 3]]


def kernel(**inputs):
    x = np.asarray(inputs["x"], np.float32)
    mem = np.asarray(inputs["mem"], np.float32)
    w = _prep_weights(inputs)
    xT = [np.ascontiguousarray(x[b].T) for b in range(8)]
    memT = [np.ascontiguousarray(mem[b].T) for b in range(8)]
    for grp in LAUNCH_GROUPS:
        xT = _run(grp, xT, memT, w)
    return np.stack([np.ascontiguousarray(y.T) for y in xT], axis=0).astype(np.float32)
```

```python
import numpy as np
from contextlib import ExitStack
import concourse.bass as bass
import concourse.mybir as mybir
from concourse.bass_utils import run_bass_kernel_spmd

F32 = mybir.dt.float32
BF16 = mybir.dt.bfloat16
AF = mybir.ActivationFunctionType
ALU = mybir.AluOpType

T = 2048
D = 1024
KC = 8
NMEM = 256
TTM = 256
TTF = 512
NPAR = 240
ALPHA = float(8.0 ** 0.25)
EPS = 1e-5
QSCALE = float(96.0 ** -0.5)
GROUPS = [(0, 9), (9, 9), (18, 4)]
USZ = 20608
NDMA = 28
NDMA_SP = 8
NIN = {0: 2576, 1: 2560}
NKO = {0: 10, 1: 8}


class Trk:
    def __init__(self, nc, es):
        self.nc = nc
        self.engs = {"pe": nc.tensor, "act": nc.scalar, "dve": nc.vector,
                     "pool": nc.gpsimd, "sp": nc.sync}
        self.sems = {k: es.enter_context(nc.semaphore("c_" + k)) for k in self.engs}
        self.cnt = {k: 0 for k in self.engs}
        self.seen = {k: {} for k in self.engs}
        self.lastw = {}
        self.readers = {}
        self.dsem = [es.enter_context(nc.semaphore("d%d" % i)) for i in range(NDMA)]
        self.dval = [0] * NDMA
        self.dnext = {}
        self.nops = 0

    def _semh(self, key):
        if isinstance(key, tuple):
            return self.dsem[key[1]]
        return self.sems[key]

    def _wait(self, eng, tok):
        key, val = tok
        if key == "pe" and eng == "pe":
            return
        if self.seen[eng].get(key, 0) >= val:
            return
        self.seen[eng][key] = val
        self.engs[eng].wait_ge(self._semh(key), val)

    def _deps(self, eng, reads, writes):
        for r in reads:
            if r in self.lastw:
                self._wait(eng, self.lastw[r])
        for w in writes:
            if w in self.lastw:
                self._wait(eng, self.lastw[w])
            for k, v in self.readers.get(w, {}).items():
                self._wait(eng, (k, v))

    def _commit(self, tok, reads, writes):
        for r in reads:
            d = self.readers.setdefault(r, {})
            if d.get(tok[0], 0) < tok[1]:
                d[tok[0]] = tok[1]
        for w in writes:
            self.lastw[w] = tok
            self.readers[w] = {}

    def op(self, eng, fn, reads=(), writes=(), sig=True):
        self._deps(eng, reads, writes)
        ins = fn(self.engs[eng])
        self.nops += 1
        if sig:
            self.cnt[eng] += 1
            tok = (eng, self.cnt[eng])
            ins.then_inc(self.sems[eng], 1)
        else:
            tok = (eng, self.cnt[eng] + 1)
        self._commit(tok, reads, writes)
        return tok

    def dma(self, q, out, in_, reads=(), writes=()):
        self._deps(q, reads, writes)
        lo, hi = (0, NDMA_SP) if q == "sp" else (NDMA_SP, NDMA)
        i = lo + self.dnext.get(q, 0)
        self.dnext[q] = (self.dnext.get(q, 0) + 1) % (hi - lo)
        if self.dval[i] > 0:
            self._wait(q, (("d", i), self.dval[i]))
        ins = self.engs[q].dma_start(out=out, in_=in_)
        self.dval[i] += 16
        tok = (("d", i), self.dval[i])
        ins.then_inc(self.dsem[i], 16)
        self.nops += 1
        self._commit(tok, reads, writes)
        return tok

    def wait_all(self, eng, res):
        for r in res:
            if r in self.lastw:
                self._wait(eng, self.lastw[r])


class Builder:
    def __init__(self, layers):
        self.layers = list(layers)
        nc = self.nc = bass.Bass("TRN2", target_bir_lowering=False)
        self.es = ExitStack()
        es = self.es

        def dt(name, shape, kind="ExternalInput"):
            return nc.dram_tensor(name, shape, F32, kind=kind).ap()

        self.xT = dt("xT", [D, T])
        self.memT = dt("memT", [D, NMEM])
        self.consts_d = dt("consts", [128, 384])
        self.params_d = dt("params", [128, 4 * NPAR])
        self.win_d = {0: dt("win_g", [2, 128, KC * 2576]), 1: dt("win_c", [2, 128, KC * 2560])}
        self.wout_d = {0: dt("wout_g", [2, 128, 10 * 1024]), 1: dt("wout_c", [2, 128, 8 * 1024])}
        self.wa2_d = dt("wa2", [2, 17, 384])
        self.wkv_d = dt("wkv", [4, 128, KC * 512])
        self.wup_d = dt("wup", [4, 22, 128, KC * 256])
        self.wdn_d = dt("wdn", [4, 8, 128, 22 * 128])
        self.yT = dt("yT", [D, T], kind="ExternalOutput")

        def sb(name, shape, dtype):
            return es.enter_context(nc.sbuf_tensor(name, shape, dtype))

        self.x32 = sb("x32", [128, KC * T], F32)
        self.U = sb("U", [128, USZ], BF16)
        self.W2 = sb("W2", [128, 10240], BF16)
        self.MIb = sb("MIb", [128, KC * T], BF16)
        self.MIf = sb("MIf", [128, 4224], F32)
        self.consts = sb("consts_s", [128, 384], F32)
        self.params = sb("params_s", [128, 4 * NPAR], F32)
        self.ones = sb("ones", [128, 128], BF16)
        self.memTb = sb("memTb", [128, KC * NMEM], BF16)
        self.memK = sb("memK", [128, 2 * NMEM], BF16)
        self.memV = sb("memV", [128, 2 * 256], BF16)
        self.wa2 = sb("wa2s", [17, 384], F32)
        self.halo = sb("halo", [128, 16], F32)
        self.scr = sb("scr", [128, 8], F32)
        self.rcb = sb("rcb", [128, 512], F32)
        self.tmpA = sb("tmpA", [128, 512], F32)
        self.tmpB = sb("tmpB", [128, 512], F32)
        self.tmpC = sb("tmpC", [128, 512], F32)
        self.tmpD = sb("tmpD", [128, 512], F32)
        self.negmean = sb("negmean", [128, TTM], F32)
        self.rstd = sb("rstd", [128, TTM], F32)
        self.nmr = sb("nmr", [128, TTM], F32)
        self.lnz = sb("lnz", [128, 2 * KC * TTM], BF16)
        self.bg = []
        self.ps = [es.enter_context(nc.psum_tensor("ps%d" % i, [128, 512], F32)) for i in range(8)]

        self.t = Trk(nc, es)
        self.bank_ctr = {}

        tt = TTM
        o = 0

        def carve(n):
            nonlocal o
            a = o
            o += n
            return a
        a0 = carve(KC * tt)
        self.o_xbt = [a0, a0]
        self.o_qd = carve(4 * tt)
        self.o_ki = carve(4 * tt)
        self.o_kend = carve((tt // 128) * 384)
        self.o_vtok = carve((tt // 128) * 768)
        self.o_att = carve(4 * 128)
        self.o_stb = carve(4 * 2 * 192)
        self.o_memq = carve(2 * tt)
        self.o_expp = carve(2 * 2 * tt)
        self.o_mix = carve(10 * tt)
        self.o_sq = carve(2 * tt)
        self.o_wkv = self.o_qd
        assert self.o_qd + 4096 <= self.o_stb
        assert o <= KC * T, o
        o = 0
        self.f_xg = carve(tt)
        self.f_ltok = carve(384)
        self.f_et = carve(tt)
        self.f_eend = carve(384)
        self.f_state = carve(4 * 192)
        self.f_decay = carve(16)
        self.f_rrms = carve(tt)
        self.f_hs = [carve(tt), carve(tt)]
        self.f_ch = [carve(tt + 2), carve(tt + 2)]
        assert o <= 4224, o
        self.lay = {0: {k: getattr(self, k) for k in ("o_xbt", "o_memq", "o_expp", "o_mix", "o_wkv", "f_hs", "f_ch")}}
        c512 = 512
        self.lay[1] = {"o_xbt": [0, 0], "o_memq": KC * c512, "o_expp": KC * c512 + 2 * c512,
                       "o_mix": KC * c512 + 2 * c512 + 4 * c512, "o_wkv": KC * c512 + 2 * c512 + 4 * c512 + 8 * c512,
                       "f_hs": [0, c512], "f_ch": [2 * c512, 3 * c512 + 2]}
        assert self.lay[1]["o_wkv"] + 4096 <= KC * T and 4 * c512 + 4 <= 4224
        self.f_ug = 0
        self.f_uv = 2 + T + 62
        assert self.f_uv + 2 + T <= 4224

    def bank(self, lo=0, hi=8):
        k = (lo, hi)
        c = self.bank_ctr.get(k, 0)
        self.bank_ctr[k] = c + 1
        return lo + (c % (hi - lo))

    def mm_group(self, out_ap, out_res, pairs, reads):
        n = len(pairs)
        for i, (l, r) in enumerate(pairs):
            self.t.op("pe", lambda e, l=l, r=r, i=i: e.matmul(out_ap, l, r, start=(i == 0), stop=(i == n - 1)),
                      reads=reads, writes=[out_res], sig=(i == n - 1))

    def par(self, l, col, rows=128):
        c = l * NPAR + col
        return self.params[0:rows, c:c + 1]

    def barrier(self, toks):
        self.t.op("dve", lambda e: e.memset(self.scr[:, 0:1], 0.0), reads=[], writes=list(toks) + ["scr"])

    def prologue(self):
        t = self.t
        t.dma("sp", self.consts[:], self.consts_d, writes=["consts"])
        t.dma("sp", self.params[:], self.params_d, writes=["params"])
        xv = self.xT.rearrange("(kc p) t -> p kc t", p=128)
        x3 = self.x32[:].rearrange("p (kc t) -> p kc t", kc=KC)
        t.dma("sp", x3[:, :, 0:TTM], xv[:, :, 0:TTM], writes=[("x32", 0)])
        self.x_rest = [(x3[:, :, q * TTM:(q + 1) * TTM], xv[:, :, q * TTM:(q + 1) * TTM], q) for q in range(1, T // TTM)]
        mv = self.memT.rearrange("(kc p) m -> p kc m", p=128)
        t.dma("pool", self.memTb[:].rearrange("p (kc m) -> p kc m", kc=KC), mv, writes=["memTb"])
        t.op("dve", lambda e: e.memset(self.ones[:], 1.0), writes=["ones"])
        t.op("dve", lambda e: e.memset(self.scr[:], 0.0), writes=["scr", "scr1"])

    def ublk(self, lo, hi):
        return [("U", q) for q in range(lo // 1024, (hi - 1) // 1024 + 1)]

    def load_win(self, l, pieces):
        t = self.t
        typ = l % 2
        j = l // 2
        nin = NIN[typ]
        step = KC * nin // 4
        for pc in pieces:
            t.dma("pool", self.U[:, pc * step:(pc + 1) * step], self.win_d[typ][j, :, pc * step:(pc + 1) * step],
                  reads=[], writes=self.ublk(pc * step, (pc + 1) * step))

    def load_wout(self, l):
        t = self.t
        typ = l % 2
        j = l // 2
        nko = NKO[typ]
        half = nko * 1024 // 2
        for pc in range(2):
            t.dma("pool", self.W2[:, pc * half:(pc + 1) * half], self.wout_d[typ][j, :, pc * half:(pc + 1) * half],
                  reads=[], writes=(["W2_own"] if pc == 0 else []) + [("wout", pc)])
        if typ == 0:
            t.dma("sp", self.wa2[:], self.wa2_d[j], writes=["wa2"])

    def win(self, typ, kc, c0, n):
        nin = NIN[typ]
        a = kc * nin + c0
        return self.U[:, a:a + n]

    def win_res(self, typ, c0=None, n=None):
        nin = NIN[typ]
        r = []
        for kc in range(KC):
            for x in self.ublk(kc * nin + c0, kc * nin + c0 + n):
                if x not in r:
                    r.append(x)
        return r

    def wout(self, typ, i, o, rows):
        a = i * 1024 + o * 128
        return self.W2[0:rows, a:a + 128]

    def mem_kv(self, l):
        t = self.t
        wk = self.MIb[:, self.o_wkv:self.o_wkv + 4096]
        t.dma("pool", wk, self.wkv_d[l], reads=["MI_own"], writes=["wkv"])
        for hp in range(2):
            b = self.bank()
            pairs = [(self.MIb[:, self.o_wkv + kc * 512 + hp * 128: self.o_wkv + kc * 512 + hp * 128 + 128],
                      self.memTb[:, kc * NMEM:(kc + 1) * NMEM]) for kc in range(KC)]
            self.mm_group(self.ps[b][:, 0:NMEM], ("ps", b), pairs, ["wkv", "memTb", "MI_own"])
            t.op("act", lambda e, b=b, hp=hp: e.copy(self.memK[:, hp * NMEM:(hp + 1) * NMEM], self.ps[b][:, 0:NMEM]),
                 reads=[("ps", b)], writes=["memK"])
        for mc in range(2):
            b = self.bank()
            pairs = [(self.memTb[:, kc * NMEM + mc * 128: kc * NMEM + mc * 128 + 128],
                      self.MIb[:, self.o_wkv + kc * 512 + 256: self.o_wkv + kc * 512 + 512]) for kc in range(KC)]
            self.mm_group(self.ps[b][:, 0:256], ("ps", b), pairs, ["wkv", "memTb", "MI_own"])
            t.op("act", lambda e, b=b, mc=mc: e.copy(self.memV[:, mc * 256:(mc + 1) * 256], self.ps[b][:, 0:256]),
                 reads=[("ps", b)], writes=["memV"])

    def inproj_fm(self, typ, c0, m, tt, out_ap, out_res):
        xo = self.o_xbt[self.xb]
        pairs = [(self.win(typ, kc, c0, m), self.MIb[:, xo + kc * tt: xo + (kc + 1) * tt])
                 for kc in range(KC)]
        self.mm_group(out_ap, out_res, pairs, self.win_res(typ, c0, m) + ["xbt", "MI_own"])
        self.tick()

    def inproj_tm(self, typ, c0, n, tt, c, out_ap, out_res):
        xo = self.o_xbt[self.xb]
        pairs = [(self.MIb[:, xo + kc * tt + c * 128: xo + kc * tt + (c + 1) * 128],
                  self.win(typ, kc, c0, n)) for kc in range(KC)]
        self.mm_group(out_ap, out_res, pairs, self.win_res(typ, c0, n) + ["xbt", "MI_own"])
        self.tick()

    def cast_tile(self, ti, tt):
        t = self.t
        t0 = ti * tt
        self.xb = ti % 2
        xo = self.o_xbt[self.xb]
        for kc in range(KC):
            src = self.x32[:, kc * T + t0: kc * T + t0 + tt]
            dst = self.MIb[:, xo + kc * tt: xo + (kc + 1) * tt]
            eng = "pool" if kc % 2 == 0 else "act"
            if eng == "act":
                t.op("act", lambda e, s=src, d=dst: e.copy(d, s), reads=self.xr(ti, tt) + ["MI_own"], writes=["xbt"])
            else:
                t.op("pool", lambda e, s=src, d=dst: e.tensor_copy(d, s), reads=self.xr(ti, tt) + ["MI_own"], writes=["xbt"])

    def xr(self, ti, tt):
        return [("x32", (ti * tt) // TTM + i) for i in range(tt // TTM)]

    def tick(self, k=1):
        if getattr(self, "no_tick", False):
            return
        for _ in range(k):
            if not self.bg:
                return
            try:
                next(self.bg[0])
            except StopIteration:
                self.bg.pop(0)

    def drain(self, upto=None):
        while self.bg:
            if upto is not None and upto not in self.bg:
                return
            try:
                next(self.bg[0])
            except StopIteration:
                self.bg.pop(0)

    def ln_task(self, l, gcol, bcol, ti):
        t = self.t
        n = TTM
        t0 = ti * n
        xres = [("x32", ti)]
        for o in range(KC):
            z = self.x32[:, o * T + t0: o * T + t0 + n]
            zb = self.lnz[:, o * n:(o + 1) * n]
            zq = self.lnz[:, (KC + o) * n:(KC + o + 1) * n]
            t.op("dve", lambda e, z=z, zb=zb: e.tensor_copy(zb, z), reads=xres, writes=["lnzb"])
            t.op("act", lambda e, z=z, zq=zq: e.activation(out=zq, in_=z, func=AF.Square), reads=xres, writes=["lnzq"])
            yield
        for _ in range(3):
            yield
        bs = 7
        pm = [(self.ones[:], self.lnz[:, o * n:(o + 1) * n]) for o in range(KC)]
        pq = [(self.ones[:], self.lnz[:, (KC + o) * n:(KC + o + 1) * n]) for o in range(KC)]
        self.mm_group(self.ps[bs][:, 0:n], ("ps", bs), pm, ["ones", "lnzb"])
        yield
        self.mm_group(self.ps[bs][:, n:2 * n], ("ps", bs), pq, ["ones", "lnzq"])
        yield
        yield
        nm = self.negmean[:, 0:n]
        rs = self.rstd[:, 0:n]
        nr = self.nmr[:, 0:n]
        t.op("act", lambda e: e.mul(nm, self.ps[bs][:, 0:n], -1.0 / D), reads=[("ps", bs)], writes=["negmean"])
        yield
        t.op("act", lambda e: e.activation(out=nr, in_=nm, func=AF.Square), reads=["negmean"], writes=["nmr"])
        yield
        t.op("dve", lambda e: e.scalar_tensor_tensor(out=rs, in0=self.ps[bs][:, n:2 * n], scalar=1.0 / D, in1=nr,
                                                      op0=ALU.mult, op1=ALU.subtract),
             reads=[("ps", bs), "nmr"], writes=["rstd"])
        yield
        t.op("act", lambda e: e.activation(out=rs, in_=rs, func=AF.Ln, bias=EPS, scale=1.0), reads=["rstd"], writes=["rstd"])
        yield
        t.op("act", lambda e: e.activation(out=rs, in_=rs, func=AF.Exp, scale=-0.5), reads=["rstd"], writes=["rstd"])
        yield
        t.op("dve", lambda e: e.tensor_tensor(out=nr, in0=nm, in1=rs, op=ALU.mult), reads=["negmean", "rstd"], writes=["nmr"])
        yield
        for o in range(KC):
            z = self.x32[:, o * T + t0: o * T + t0 + n]
            g = self.par(l, gcol + o)
            bb = self.par(l, bcol + o)
            cr = ("x32c", ti, o)
            t.op("dve", lambda e, z=z: e.tensor_tensor(out=z, in0=z, in1=rs, op=ALU.mult), reads=xres + ["rstd"], writes=[cr])
            t.op("dve", lambda e, z=z: e.tensor_tensor(out=z, in0=z, in1=nr, op=ALU.add), reads=["nmr"], writes=[cr])
            yield
            t.op("act", lambda e, z=z, g=g, bb=bb: e.activation(out=z, in_=z, func=AF.Identity, bias=bb, scale=g),
                 reads=["params"], writes=[cr])
            yield
        t.op("act", lambda e: e.copy(self.scr[:, 1:2], self.scr[:, 2:3]), reads=[("x32c", ti, o) for o in range(KC)],
             writes=xres + ["scr1"])

    def outproj_residual(self, typ, ti, tt):
        t = self.t
        t0 = ti * tt
        nko = NKO[typ]
        for o in range(KC):
            b = self.bank(0, 7)
            pairs = []
            for i in range(nko):
                rows = 96 if (typ == 0 and i < 8) else 128
                pairs.append((self.wout(typ, i, o, rows), self.MIb[0:rows, self.o_mix + i * tt: self.o_mix + (i + 1) * tt]))
            self.mm_group(self.ps[b][:, 0:tt], ("ps", b), pairs, ["W2_own", ("wout", 0), ("wout", 1), "mixT", "MI_own"])
            z = self.x32[:, o * T + t0: o * T + t0 + tt]
            t.op("dve", lambda e, z=z, b=b: e.scalar_tensor_tensor(out=z, in0=z, scalar=ALPHA, in1=self.ps[b][:, 0:tt],
                                                                  op0=ALU.mult, op1=ALU.add),
                 reads=[("ps", b)] + self.xr(ti, tt), writes=self.xr(ti, tt))
            self.tick()

    def xattn_gen(self, typ, ti, tt, memq_c0, lo):
        t = self.t
        for hp in range(2):
            b = self.bank(lo, 7)
            self.inproj_fm(typ, memq_c0 + hp * 128, 128, tt, self.ps[b][:, 0:tt], ("ps", b))
            t.op("act", lambda e, b=b, hp=hp: e.copy(self.MIb[:, self.o_memq + hp * tt: self.o_memq + (hp + 1) * tt], self.ps[b][:, 0:tt]),
                 reads=[("ps", b), "MI_own"], writes=[("memq", hp)])
            yield

        def s_stage(h):
            hp, hh = h // 2, h % 2
            r0 = 64 * hh
            eb = h % 2
            for mc in range(2):
                b = self.bank(lo, 7)
                t.op("pe", lambda e, b=b, hp=hp, mc=mc, r0=r0: e.matmul(
                    self.ps[b][:, 0:tt],
                    self.memK[r0:r0 + 64, hp * NMEM + mc * 128: hp * NMEM + mc * 128 + 128],
                    self.MIb[r0:r0 + 64, self.o_memq + hp * tt: self.o_memq + (hp + 1) * tt],
                    start=True, stop=True),
                    reads=["memK", ("memq", hp), "MI_own"], writes=[("ps", b)])
                dst = self.MIb[:, self.o_expp + (eb * 2 + mc) * tt: self.o_expp + (eb * 2 + mc + 1) * tt]
                t.op("act", lambda e, b=b, dst=dst: e.activation(out=dst, in_=self.ps[b][:, 0:tt], func=AF.Exp, scale=0.125),
                     reads=[("ps", b), "MI_own"], writes=[("expp", eb, mc)])

        def pv_stage(h):
            hp, hh = h // 2, h % 2
            r0 = 64 * hh
            eb = h % 2
            bo = self.bank(lo, 7)
            bs = self.bank(lo, 7)
            pairs_o, pairs_s = [], []
            for mc in range(2):
                ep = self.MIb[:, self.o_expp + (eb * 2 + mc) * tt: self.o_expp + (eb * 2 + mc + 1) * tt]
                pairs_o.append((self.memV[:, mc * 256 + hp * 128: mc * 256 + hp * 128 + 128], ep))
                pairs_s.append((self.ones[:], ep))
            rd = ["memV", "ones", ("expp", eb, 0), ("expp", eb, 1), "MI_own"]
            self.mm_group(self.ps[bo][:, 0:tt], ("ps", bo), pairs_o, rd)
            self.mm_group(self.ps[bs][:, 0:tt], ("ps", bs), pairs_s, rd)
            rc = self.rcb[r0:r0 + 64, 0:tt]
            t.op("act", lambda e, bs=bs, rc=rc, r0=r0: e.activation(out=rc, in_=self.ps[bs][r0:r0 + 64, 0:tt], func=AF.Ln),
                 reads=[("ps", bs)], writes=["rcb"])
            t.op("act", lambda e, rc=rc: e.activation(out=rc, in_=rc, func=AF.Exp, scale=-1.0), reads=["rcb"], writes=["rcb"])
            i = NKO[typ] - 2 + hp
            dst = self.MIb[r0:r0 + 64, self.o_mix + i * tt: self.o_mix + (i + 1) * tt]
            t.op("dve", lambda e, bo=bo, rc=rc, dst=dst, r0=r0: e.tensor_tensor(out=dst, in0=self.ps[bo][r0:r0 + 64, 0:tt], in1=rc, op=ALU.mult),
                 reads=[("ps", bo), "rcb", "MI_own"], writes=["mixT"])

        s_stage(0)
        yield
        for h in range(4):
            if h + 1 < 4:
                s_stage(h + 1)
                yield
            pv_stage(h)
            self.tick()
            yield

    def fill(self, k=1):
        for _ in range(k):
            if self.filler is None:
                return
            try:
                next(self.filler)
            except StopIteration:
                self.filler = None

    def fill_all(self):
        while self.filler is not None:
            self.fill()

    def conv_tile(self, l, ti, tt):
        t = self.t
        typ = 1
        PC = 208

        def stage1(ci):
            p = ci % 2
            hsn, chn = ("hs", p), ("ch", p)
            bh = self.bank(0, 7)
            self.inproj_fm(typ, 1536 + 128 * ci, 128, tt, self.ps[bh][:, 0:tt], ("ps", bh))
            hs = self.MIf[:, self.f_hs[p]:self.f_hs[p] + tt]
            t.op("act", lambda e, bh=bh, hs=hs: e.copy(hs, self.ps[bh][:, 0:tt]), reads=[("ps", bh), "MI_own"], writes=[hsn])
            bc = self.bank(0, 7)
            self.inproj_fm(typ, 768 + 128 * ci, 128, tt, self.ps[bc][:, 0:tt], ("ps", bc))
            ch = self.MIf[:, self.f_ch[p]:self.f_ch[p] + tt + 2]
            t.op("act", lambda e, ci=ci, ch=ch: e.copy(ch[:, 0:2], self.halo[:, 2 * ci:2 * ci + 2]),
                 reads=[("halo", ci), "MI_own"], writes=[chn])
            t.op("dve", lambda e, bc=bc, hs=hs, ch=ch: e.tensor_tensor(out=ch[:, 2:tt + 2], in0=self.ps[bc][:, 0:tt], in1=hs, op=ALU.mult),
                 reads=[("ps", bc), hsn, "MI_own"], writes=[chn])

        def stage2(ci):
            p = ci % 2
            chn = ("ch", p)
            an = "tmpA" if p == 0 else "tmpB"
            ch = self.MIf[:, self.f_ch[p]:self.f_ch[p] + tt + 2]
            t.op("act", lambda e, ci=ci, ch=ch: e.copy(self.halo[:, 2 * ci:2 * ci + 2], ch[:, tt:tt + 2]),
                 reads=[chn], writes=[("halo", ci)])
            a = (self.tmpA if p == 0 else self.tmpB)[:, 0:tt]
            w0, w1, w2 = (self.par(l, PC + ci * 3 + k) for k in range(3))
            t.op("act", lambda e, a=a, ch=ch, w2=w2: e.mul(a, ch[:, 2:tt + 2], w2), reads=[chn, "params"], writes=[an])
            t.op("dve", lambda e, a=a, ch=ch, w1=w1: e.scalar_tensor_tensor(out=a, in0=ch[:, 1:tt + 1], scalar=w1, in1=a, op0=ALU.mult, op1=ALU.add),
                 reads=[chn, "params", an], writes=[an])
            t.op("dve", lambda e, a=a, ch=ch, w0=w0: e.scalar_tensor_tensor(out=a, in0=ch[:, 0:tt], scalar=w0, in1=a, op0=ALU.mult, op1=ALU.add),
                 reads=[chn, "params", an], writes=[an])
            bb = self.bank(0, 7)
            self.inproj_fm(typ, 128 * ci, 128, tt, self.ps[bb][:, 0:tt], ("ps", bb))
            dst = self.MIb[:, self.o_mix + ci * tt: self.o_mix + (ci + 1) * tt]
            t.op("dve", lambda e, bb=bb, a=a, dst=dst: e.tensor_tensor(out=dst, in0=self.ps[bb][:, 0:tt], in1=a, op=ALU.mult),
                 reads=[("ps", bb), an, "MI_own"], writes=["mixT"])
            self.tick(5)

        stage1(0)
        for ci in range(6):
            if ci + 1 < 6:
                stage1(ci + 1)
            stage2(ci)

    def gla_tile(self, l, ti, tt):
        t = self.t
        typ = 0
        nch = tt // 128
        triF = self.consts[:, 0:128]
        triR = self.consts[:, 128:256]
        maskT = self.consts[:, 256:384]
        PH = 226

        def gps(h):
            return self.ps[h // 2][0:96, (h % 2) * tt:(h % 2 + 1) * tt]
        b = self.bank(2, 4)
        self.inproj_fm(typ, 2304, 16, tt, self.ps[b][0:16, 0:tt], ("ps", b))
        xg = self.MIf[0:17, self.f_xg:self.f_xg + tt]
        t.op("act", lambda e, b=b: e.copy(self.MIf[0:16, self.f_xg:self.f_xg + tt], self.ps[b][0:16, 0:tt]),
             reads=[("ps", b), "MI_own"], writes=["xg"])
        ltok = self.MIf[:, self.f_ltok:self.f_ltok + 384]
        eend = self.MIf[:, self.f_eend:self.f_eend + 384]
        for c in range(nch):
            b = self.bank(2, 4)
            t.op("pe", lambda e, b=b, c=c: e.matmul(self.ps[b][:, 0:384], xg[:, c * 128:(c + 1) * 128], self.wa2[:, :], start=True, stop=True),
                 reads=["xg", "wa2", "MI_own"], writes=[("ps", b)])
            t.op("act", lambda e, b=b: e.activation(out=ltok, in_=self.ps[b][:, 0:384], func=AF.Exp, scale=-1.0),
                 reads=[("ps", b), "MI_own"], writes=["ltok"])
            t.op("act", lambda e: e.activation(out=ltok, in_=ltok, func=AF.Ln, bias=1.0, scale=1.0), reads=["ltok"], writes=["ltok"])
            for half in range(2):
                b4 = self.bank(4, 7)
                self.inproj_tm(typ, 768 + 384 * half, 384, tt, c, self.ps[b4][:, 0:384], ("ps", b4))
                vd = self.MIb[:, self.o_vtok + c * 768 + half * 384: self.o_vtok + c * 768 + (half + 1) * 384]
                t.op("act", lambda e, b4=b4, vd=vd: e.copy(vd, self.ps[b4][:, 0:384]),
                     reads=[("ps", b4), "MI_own"], writes=[("vtok", c)])
            for h in range(4):
                t.op("pe", lambda e, h=h, c=c: e.matmul(gps(h)[:, c * 128:(c + 1) * 128], ltok[:, 96 * h:96 * h + 96], triF, start=True, stop=True),
                     reads=["ltok", "consts", "MI_own"], writes=[("ps", h // 2)])
            b2 = self.bank(2, 4)
            t.op("pe", lambda e, b2=b2: e.matmul(self.ps[b2][:, 0:384], triR, ltok, start=True, stop=True),
                 reads=["ltok", "consts", "MI_own"], writes=[("ps", b2)])
            t.op("act", lambda e, b2=b2: e.activation(out=eend, in_=self.ps[b2][:, 0:384], func=AF.Exp),
                 reads=[("ps", b2), "MI_own"], writes=["eend"])
            b3 = self.bank(4, 7)
            self.inproj_tm(typ, 384, 384, tt, c, self.ps[b3][:, 0:384], ("ps", b3))
            kd = self.MIb[:, self.o_kend + c * 384: self.o_kend + (c + 1) * 384]
            t.op("dve", lambda e, b3=b3, kd=kd: e.tensor_tensor(out=kd, in0=self.ps[b3][:, 0:384], in1=eend, op=ALU.mult),
                 reads=[("ps", b3), "eend", "MI_own"], writes=[("kend", c)])
        et = self.MIf[0:96, self.f_et:self.f_et + tt]
        for h in range(4):
            bq = self.bank(4, 7)
            self.inproj_fm(typ, 96 * h, 96, tt, self.ps[bq][0:96, 0:tt], ("ps", bq))
            bk = self.bank(4, 7)
            self.inproj_fm(typ, 384 + 96 * h, 96, tt, self.ps[bk][0:96, 0:tt], ("ps", bk))
            t.op("act", lambda e, h=h: e.activation(out=et, in_=gps(h), func=AF.Exp),
                 reads=[("ps", h // 2), "MI_own"], writes=["et"])
            dec = self.MIf[0:96, self.f_decay + h * 4: self.f_decay + h * 4 + nch]
            t.op("dve", lambda e, dec=dec: e.tensor_copy(dec, et[:, 127:tt:128]), reads=["et"], writes=[("decay", h)])
            qd = self.MIb[0:96, self.o_qd + h * tt: self.o_qd + (h + 1) * tt]
            t.op("dve", lambda e, bq=bq, qd=qd: e.scalar_tensor_tensor(out=qd, in0=self.ps[bq][0:96, 0:tt], scalar=QSCALE, in1=et,
                                                                       op0=ALU.mult, op1=ALU.mult),
                 reads=[("ps", bq), "et", "MI_own"], writes=[("qd", h)])
            einv = self.tmpD[0:96, 0:tt]
            t.op("act", lambda e, h=h, einv=einv: e.activation(out=einv, in_=gps(h), func=AF.Exp, scale=-1.0),
                 reads=[("ps", h // 2), "MI_own"], writes=["tmpD"])
            ki = self.MIb[0:96, self.o_ki + h * tt: self.o_ki + (h + 1) * tt]
            t.op("dve", lambda e, bk=bk, ki=ki, einv=einv: e.tensor_tensor(out=ki, in0=self.ps[bk][0:96, 0:tt], in1=einv, op=ALU.mult),
                 reads=[("ps", bk), "tmpD", "MI_own"], writes=[("ki", h)])
        self.filler = self.xattn_gen(typ, ti, tt, 2320, 2)
        tmps = [(self.tmpA, "tmpA"), (self.tmpB, "tmpB"), (self.tmpC, "tmpC"), (self.tmpD, "tmpD")]
        for hpair in range(2):
            heads = (2 * hpair, 2 * hpair + 1)
            srs = {}
            self.no_tick = True
            for h in heads:
                for vt in range(2):
                    i8 = 2 * h + vt
                    br = self.bank(4, 7)
                    self.inproj_fm(typ, 1536 + 96 * i8, 96, tt, self.ps[br][0:96, 0:tt], ("ps", br))
                    tb, tn = tmps[(h % 2) * 2 + vt]
                    sr = tb[0:96, 0:tt]
                    t.op("act", lambda e, br=br, sr=sr: e.activation(out=sr, in_=self.ps[br][0:96, 0:tt], func=AF.Silu),
                         reads=[("ps", br)], writes=[tn])
                    srs[(h, vt)] = (sr, tn)
            self.no_tick = False
            self.fill()
            for c in range(nch):
                gchunk = ti * nch + c
                cur = gchunk % 2
                nxt = 1 - cur
                cs = slice(c * 128, (c + 1) * 128)
                bas, bkvs = {}, {}
                for h in heads:
                    qd = self.MIb[0:96, self.o_qd + h * tt: self.o_qd + (h + 1) * tt]
                    ki = self.MIb[0:96, self.o_ki + h * tt: self.o_ki + (h + 1) * tt]
                    ba = self.bank(2, 4)
                    t.op("pe", lambda e, ba=ba, cs=cs, ki=ki, qd=qd: e.matmul(self.ps[ba][:, 0:128], ki[:, cs], qd[:, cs], start=True, stop=True),
                         reads=[("ki", h), ("qd", h), "MI_own"], writes=[("ps", ba)])
                    bkv = self.bank(4, 7)
                    t.op("pe", lambda e, bkv=bkv, c=c, h=h: e.matmul(
                        self.ps[bkv][0:96, 0:192], self.MIb[:, self.o_kend + c * 384 + 96 * h: self.o_kend + c * 384 + 96 * h + 96],
                        self.MIb[:, self.o_vtok + c * 768 + h * 192: self.o_vtok + c * 768 + (h + 1) * 192], start=True, stop=True),
                        reads=[("kend", c), ("vtok", c), "MI_own"], writes=[("ps", bkv)])
                    bas[h], bkvs[h] = ba, bkv
                for h in heads:
                    qd = self.MIb[0:96, self.o_qd + h * tt: self.o_qd + (h + 1) * tt]
                    pb = h % 2
                    st32 = self.MIf[0:96, self.f_state + h * 192: self.f_state + (h + 1) * 192]
                    stb_cur = self.MIb[0:96, self.o_stb + (h * 2 + cur) * 192: self.o_stb + (h * 2 + cur + 1) * 192]
                    stb_nxt = self.MIb[0:96, self.o_stb + (h * 2 + nxt) * 192: self.o_stb + (h * 2 + nxt + 1) * 192]
                    ba, bkv = bas[h], bkvs[h]
                    ab = (h % 2) * 2 + c % 2
                    att = self.MIb[:, self.o_att + ab * 128: self.o_att + (ab + 1) * 128]
                    t.op("dve", lambda e, ba=ba, att=att: e.tensor_tensor(out=att, in0=self.ps[ba][:, 0:128], in1=maskT, op=ALU.mult),
                         reads=[("ps", ba), "consts", "MI_own"], writes=[("att", ab)])
                    dcol = self.f_decay + h * 4 + c
                    t.op("dve", lambda e, bkv=bkv, st32=st32, dcol=dcol: e.scalar_tensor_tensor(
                        out=st32, in0=st32, scalar=self.MIf[0:96, dcol:dcol + 1], in1=self.ps[bkv][0:96, 0:192], op0=ALU.mult, op1=ALU.add),
                        reads=[("ps", bkv), ("decay", h), ("state", h), "MI_own"], writes=[("state", h)])
                    for vt in range(2):
                        vcol = self.o_vtok + c * 768 + h * 192 + vt * 96
                        osl = slice(vt * tt + c * 128, vt * tt + (c + 1) * 128)
                        t.op("pe", lambda e, vcol=vcol, att=att, osl=osl, pb=pb: e.matmul(
                            self.ps[pb][0:96, osl], self.MIb[:, vcol:vcol + 96], att, start=True, stop=False),
                            reads=[("vtok", c), ("att", ab), "MI_own"], writes=[("ps", pb)], sig=False)
                        t.op("pe", lambda e, vt=vt, cs=cs, osl=osl, stb_cur=stb_cur, qd=qd, pb=pb: e.matmul(
                            self.ps[pb][0:96, osl], stb_cur[:, vt * 96:(vt + 1) * 96], qd[:, cs], start=False, stop=True),
                            reads=[("stb", h, cur), ("qd", h), "MI_own"], writes=[("ps", pb)])
                    t.op("act", lambda e, st32=st32, stb_nxt=stb_nxt: e.copy(stb_nxt, st32),
                         reads=[("state", h), "MI_own"], writes=[("stb", h, nxt)])
                self.tick()
                self.fill()
            for h in heads:
                pb = h % 2
                bss = self.bank(2, 4)
                sq = self.MIb[0:96, self.o_sq: self.o_sq + 2 * tt]
                t.op("act", lambda e, sq=sq, pb=pb: e.activation(out=sq, in_=self.ps[pb][0:96, 0:2 * tt], func=AF.Square),
                     reads=[("ps", pb), "MI_own"], writes=["sq"])
                for vt in range(2):
                    t.op("pe", lambda e, vt=vt, bss=bss, sq=sq: e.matmul(self.ps[bss][0:96, 0:tt], self.ones[0:96, 0:96], sq[:, vt * tt:(vt + 1) * tt], start=(vt == 0), stop=(vt == 1)),
                         reads=["sq", "ones", "MI_own"], writes=[("ps", bss)], sig=(vt == 1))
                rr = self.MIf[0:96, self.f_rrms:self.f_rrms + tt]
                t.op("act", lambda e, bss=bss, rr=rr: e.activation(out=rr, in_=self.ps[bss][0:96, 0:tt], func=AF.Ln, bias=EPS, scale=1.0 / 192.0),
                     reads=[("ps", bss), "MI_own"], writes=["rrms"])
                t.op("act", lambda e, rr=rr: e.activation(out=rr, in_=rr, func=AF.Exp, scale=-0.5), reads=["rrms"], writes=["rrms"])
                self.fill()
                for vt in range(2):
                    i8 = 2 * h + vt
                    sr, srn = srs[(h, vt)]
                    hg = self.par(l, PH + i8, rows=96)
                    t.op("dve", lambda e, sr=sr, hg=hg, rr=rr: e.scalar_tensor_tensor(out=sr, in0=sr, scalar=hg, in1=rr, op0=ALU.mult, op1=ALU.mult),
                         reads=[srn, "rrms", "params"], writes=[srn])
                    dst = self.MIb[0:96, self.o_mix + i8 * tt: self.o_mix + (i8 + 1) * tt]
                    t.op("dve", lambda e, vt=vt, sr=sr, dst=dst, pb=pb: e.tensor_tensor(out=dst, in0=self.ps[pb][0:96, vt * tt:(vt + 1) * tt], in1=sr, op=ALU.mult),
                         reads=[("ps", pb), srn, "MI_own"], writes=["mixT"])
                self.tick()

    def mixer(self, l):
        t = self.t
        typ = l % 2
        tt = TTM if typ == 0 else 512
        for k, v in self.lay[typ].items():
            setattr(self, k, v)
        if getattr(self, "memkv_done", None) != l:
            self.mem_kv(l)
        if getattr(self, "wout_pending", None) == l:
            self.wout_pending = None
            self.load_wout(l)
        if typ == 0:
            t.op("dve", lambda e: e.memset(self.MIf[0:96, self.f_state:self.f_state + 768], 0.0),
                 reads=["MI_own"], writes=[("state", h) for h in range(4)])
            t.op("dve", lambda e: e.memset(self.MIb[0:96, self.o_stb:self.o_stb + 1536], 0.0),
                 reads=["MI_own"], writes=[("stb", h, k) for h in range(4) for k in range(2)])
            t.op("dve", lambda e: e.memset(self.MIf[0:17, self.f_xg:self.f_xg + tt], 1.0), reads=["MI_own"], writes=["xg"])
        else:
            t.op("dve", lambda e: e.memset(self.halo[:], 0.0), writes=[("halo", ci) for ci in range(6)])
        for ti in range(T // tt):
            if self.ln2_tasks:
                self.drain(upto=self.ln2_tasks[(ti + 1) * tt // TTM - 1])
            self.cast_tile(ti, tt)
            if typ == 0:
                self.gla_tile(l, ti, tt)
            else:
                self.filler = self.xattn_gen(typ, ti, tt, 2304, 0)
                self.conv_tile(l, ti, tt)
            self.fill_all()
            self.outproj_residual(typ, ti, tt)
            for i in range(tt // TTM):
                self.bg.append(self.ln_task(l, 0, 8, ti * tt // TTM + i))
        self.drain()

    def load_wup(self, l, j, buf):
        self.t.dma("pool", self.W2[:, buf * 2048:(buf + 1) * 2048], self.wup_d[l, j],
                   reads=[], writes=[("wupb", buf)] + (["W2_own"] if self.first_ffn_dma else []))
        self.first_ffn_dma = False

    def ffn(self, l, next_l):
        t = self.t
        tt = TTF
        ntile = T // tt
        PW, PB = 32, 164
        self.first_ffn_dma = True
        nwb = 3
        units = [(gi, jj) for gi, (s0, n) in enumerate(GROUPS) for jj in range(n)]
        for k in range(nwb - 1):
            gi, jj = units[k]
            self.load_wup(l, GROUPS[gi][0] + jj, k % nwb)
        self.barrier(["MI_own"])
        xr = [("x32", i) for i in range(T // TTM)]
        k = 0
        for hf in range(2):
            for kc in range(KC):
                src = self.x32[:, kc * T + hf * 1024: kc * T + (hf + 1) * 1024]
                dst = self.MIb[:, kc * T + hf * 1024: kc * T + (hf + 1) * 1024]
                xrh = [("x32", i) for i in range(hf * 1024 // TTM, (hf + 1) * 1024 // TTM)]
                if k % 2 == 0:
                    t.op("act", lambda e, s=src, d=dst: e.copy(d, s), reads=xrh + ["MI_own"], writes=[("xbf", kc, hf)])
                else:
                    t.op("dve", lambda e, s=src, d=dst: e.tensor_copy(d, s), reads=xrh + ["MI_own"], writes=[("xbf", kc, hf)])
                k += 1
        ug = self.MIf[:, self.f_ug:self.f_ug + 2 + T]
        uv = self.MIf[:, self.f_uv:self.f_uv + 2 + T]
        t.op("dve", lambda e: e.memset(ug[:, 0:2], 0.0), reads=["MI_own"], writes=[("ug", -1)])
        t.op("dve", lambda e: e.memset(uv[:, 0:2], 0.0), reads=["MI_own"], writes=[("uv", -1)])
        agb = [(self.tmpA, "tmpA"), (self.tmpC, "tmpC")]
        avb = [(self.tmpB, "tmpB"), (self.tmpD, "tmpD")]
        pend = []
        ucount = 0
        WDN0 = 6144
        last_gi = len(GROUPS) - 1

        def stage_b(item):
            jj, t0, p = item
            ag, agn = agb[p]
            av, avn = avb[p]
            t.op("act", lambda e, ag=ag: e.activation(out=ag[:, 0:tt], in_=ag[:, 0:tt], func=AF.Silu), reads=[agn], writes=[agn])
            hdst = self.U[:, jj * T + t0: jj * T + t0 + tt]
            t.op("pool", lambda e, ag=ag, av=av, hdst=hdst: e.tensor_tensor(out=hdst, in0=ag[:, 0:tt], in1=av[:, 0:tt], op=ALU.mult),
                 reads=[agn, avn], writes=self.ublk(jj * T + t0, jj * T + t0 + tt))

        def wdn_names(slot_off, n):
            return [("wdn", q) for q in range(slot_off // 1024, (slot_off + n * 128 - 1) // 1024 + 1)]

        def load_wdn(gi, o, slot_off, n):
            s0 = GROUPS[gi][0]
            t.dma("pool", self.W2[:, WDN0 + slot_off: WDN0 + slot_off + n * 128], self.wdn_d[l, o, :, s0 * 128:(s0 + n) * 128],
                  reads=[], writes=wdn_names(slot_off, n))

        def wdn_names(slot_off, n):
            return [("wdn", q) for q in range(slot_off // 1024, (slot_off + n * 128 - 1) // 1024 + 1)]

        for ui, (gi, jj) in enumerate(units):
            s0, gn = GROUPS[gi]
            j = s0 + jj
            buf = ui % nwb
            if ui + nwb - 1 < len(units):
                g2, jj2 = units[ui + nwb - 1]
                self.load_wup(l, GROUPS[g2][0] + jj2, (ui + nwb - 1) % nwb)
            if jj == 0:
                if gi < last_gi:
                    load_wdn(gi, 0, 0, gn)
                    load_wdn(gi, 1, 2048, gn)
                else:
                    for o in range(KC):
                        load_wdn(gi, o, o * gn * 128, gn)
            wg = [self.par(l, PW + j * 3 + k) for k in range(3)]
            wv = [self.par(l, PW + (22 + j) * 3 + k) for k in range(3)]
            bgp = self.par(l, PB + j)
            bvp = self.par(l, PB + 22 + j)
            for ti in range(ntile):
                t0 = ti * tt
                p = ucount % 2
                ucount += 1
                bg = self.bank(0, 7)
                bv = self.bank(0, 7)
                xbf_res = [("xbf", kc, t0 // 1024) for kc in range(KC)] + ["MI_own"]
                for which, bnk in ((0, bg), (1, bv)):
                    pairs = [(self.W2[:, buf * 2048 + kc * 256 + which * 128: buf * 2048 + kc * 256 + which * 128 + 128],
                              self.MIb[:, kc * T + t0: kc * T + t0 + tt]) for kc in range(KC)]
                    self.mm_group(self.ps[bnk][:, 0:tt], ("ps", bnk), pairs, [("wupb", buf), "W2_own"] + xbf_res)
                ag, agn = agb[p]
                av, avn = avb[p]
                paths = ((ug, "ug", bg, ag[:, 0:tt], agn, wg, bgp), (uv, "uv", bv, av[:, 0:tt], avn, wv, bvp))
                for (u, un, bnk, a, ares, w, bp) in paths:
                    t.op("act", lambda e, u=u, bnk=bnk: e.copy(u[:, 2 + t0:2 + t0 + tt], self.ps[bnk][:, 0:tt]),
                         reads=[("ps", bnk), "MI_own"], writes=[(un, ti)])
                if pend:
                    stage_b(pend.pop(0))
                for (u, un, bnk, a, ares, w, bp) in paths:
                    ur = [(un, ti), (un, ti - 1)]
                    t.op("act", lambda e, a=a, bnk=bnk, w=w, bp=bp: e.activation(out=a, in_=self.ps[bnk][:, 0:tt], func=AF.Identity, bias=bp, scale=w[2]),
                         reads=[("ps", bnk), "params"], writes=[ares])
                    t.op("dve", lambda e, a=a, u=u, w=w: e.scalar_tensor_tensor(out=a, in0=u[:, 1 + t0:1 + t0 + tt], scalar=w[1], in1=a, op0=ALU.mult, op1=ALU.add),
                         reads=ur + ["params", ares, "MI_own"], writes=[ares])
                    t.op("dve", lambda e, a=a, u=u, w=w: e.scalar_tensor_tensor(out=a, in0=u[:, t0:t0 + tt], scalar=w[0], in1=a, op0=ALU.mult, op1=ALU.add),
                         reads=ur + ["params", ares, "MI_own"], writes=[ares])
                pend.append((jj, t0, p))
            if jj == gn - 1:
                while pend:
                    stage_b(pend.pop(0))
                if gi < last_gi:
                    for o in range(KC):
                        so = (o % 2) * 2048
                        for ti in range(ntile):
                            t0 = ti * tt
                            b = self.bank(0, 7)
                            pairs = [(self.W2[:, WDN0 + so + q * 128: WDN0 + so + (q + 1) * 128], self.U[:, q * T + t0: q * T + t0 + tt]) for q in range(gn)]
                            hres = []
                            for q in range(gn):
                                hres += self.ublk(q * T + t0, q * T + t0 + tt)
                            self.mm_group(self.ps[b][:, 0:tt], ("ps", b), pairs, wdn_names(so, gn) + ["W2_own"] + hres)
                            z = self.x32[:, o * T + t0: o * T + t0 + tt]
                            xres = [("x32", t0 // TTM + i) for i in range(tt // TTM)]
                            if gi == 0:
                                t.op("dve", lambda e, z=z, b=b: e.scalar_tensor_tensor(out=z, in0=z, scalar=ALPHA, in1=self.ps[b][:, 0:tt], op0=ALU.mult, op1=ALU.add),
                                     reads=[("ps", b)] + xres, writes=xres)
                            else:
                                t.op("dve", lambda e, z=z, b=b: e.tensor_tensor(out=z, in0=z, in1=self.ps[b][:, 0:tt], op=ALU.add),
                                     reads=[("ps", b)] + xres, writes=xres)
                        if o + 2 < KC:
                            load_wdn(gi, o + 2, so, gn)
                else:
                    self.barrier(["MI_own"])
                    if next_l is not None:
                        self.o_wkv = self.lay[next_l % 2]["o_wkv"]
                        self.mem_kv(next_l)
                        self.memkv_done = next_l
                        self.load_win(next_l, [2, 3])
                    self.ln2_tasks = []
                    t2 = TTM
                    for ti in range(T // t2):
                        t0 = ti * t2
                        xres = [("x32", ti)]
                        hres = []
                        for q in range(gn):
                            hres += self.ublk(q * T + t0, q * T + t0 + t2)
                        for o in range(KC):
                            so = o * gn * 128
                            b = self.bank(0, 7)
                            pairs = [(self.W2[:, WDN0 + so + q * 128: WDN0 + so + (q + 1) * 128], self.U[:, q * T + t0: q * T + t0 + t2]) for q in range(gn)]
                            self.mm_group(self.ps[b][:, 0:t2], ("ps", b), pairs, wdn_names(so, gn) + ["W2_own"] + hres)
                            z = self.x32[:, o * T + t0: o * T + t0 + t2]
                            t.op("dve", lambda e, z=z, b=b: e.tensor_tensor(out=z, in0=z, in1=self.ps[b][:, 0:t2], op=ALU.add),
                                 reads=[("ps", b)] + xres, writes=xres)
                            self.tick(5)
                        task = self.ln_task(l, 16, 24, ti)
                        self.ln2_tasks.append(task)
                        self.bg.append(task)
                    if next_l is not None:
                        self.load_win(next_l, [0, 1])
                        self.load_wout(next_l)

    def build(self):
        t = self.t
        gl = [l for l in self.layers if l % 2 == 0]
        self.first_gla = gl[0] if gl else -1
        self.ln2_tasks = []
        self.prologue()
        self.load_win(self.layers[0], [0, 1, 2, 3])
        for (dst, src, q) in self.x_rest:
            t.dma("sp", dst, src, reads=self.ublk(0, USZ), writes=[("x32", q)])
        self.wout_pending = self.layers[0]
        for li, l in enumerate(self.layers):
            nl = self.layers[li + 1] if li + 1 < len(self.layers) else None
            self.mixer(l)
            self.ffn(l, nl)
        self.drain()
        yv = self.yT.rearrange("(kc p) t -> p kc t", p=128)
        x3 = self.x32[:].rearrange("p (kc t) -> p kc t", kc=KC)
        for q in range(4):
            t.dma("sp", yv[:, :, q * 512:(q + 1) * 512], x3[:, :, q * 512:(q + 1) * 512],
                  reads=[("x32", q * 512 // TTM + i) for i in range(512 // TTM)], writes=[("y", q)])
        t.wait_all("sp", [("y", q) for q in range(4)])
        return self.nc


def _prep_weights(inp):
    f = np.float32
    out = {}
    tri = np.arange(128)
    c = np.zeros((128, 384), f)
    c[:, 0:128] = np.where(tri[:, None] <= tri[None, :], -1.0 / 16.0, 0.0)
    c[:, 128:256] = np.where(tri[:, None] > tri[None, :], -1.0 / 16.0, 0.0)
    c[:, 256:384] = np.where(tri[:, None] <= tri[None, :], 1.0, 0.0)
    out["consts"] = c
    par = np.zeros((128, 4 * NPAR), f)
    for l in range(4):
        b = l * NPAR
        j = l // 2
        for name, col in (("ln1_g", 0), ("ln1_b", 8), ("ln2_g", 16), ("ln2_b", 24)):
            par[:, b + col:b + col + 8] = np.asarray(inp[name][l]).reshape(8, 128).T
        cw = np.asarray(inp["ffn_conv_w"][l])
        par[:, b + 32:b + 32 + 132] = cw.reshape(3, 44, 128).transpose(2, 1, 0).reshape(128, 132)
        par[:, b + 164:b + 208] = np.asarray(inp["ffn_conv_b"][l]).reshape(44, 128).T
        if l % 2 == 1:
            mw = np.asarray(inp["conv_w"][j])
            par[:, b + 208:b + 226] = mw.reshape(3, 6, 128).transpose(2, 1, 0).reshape(128, 18)
        else:
            par[0:96, b + 226:b + 234] = np.asarray(inp["gla_head_g"][j]).reshape(8, 96).T
    out["params"] = par

    def kmajor(w):
        L, K, N = w.shape
        return np.ascontiguousarray(w.reshape(L, K // 128, 128, N).transpose(0, 2, 1, 3).reshape(L, 128, (K // 128) * N))
    out["win_g"] = kmajor(np.asarray(inp["gla_w_in"], f))
    out["win_c"] = kmajor(np.asarray(inp["conv_w_in"], f))
    out["wkv"] = kmajor(np.asarray(inp["w_mem_kv"], f))
    out["wout_c"] = kmajor(np.asarray(inp["conv_w_out"], f))
    gw = np.asarray(inp["gla_w_out"], f)
    wg = np.zeros((2, 128, 10, 1024), f)
    wg[:, 0:96, 0:8, :] = gw[:, 0:768, :].reshape(2, 8, 96, 1024).transpose(0, 2, 1, 3)
    wg[:, :, 8:10, :] = gw[:, 768:1024, :].reshape(2, 2, 128, 1024).transpose(0, 2, 1, 3)
    out["wout_g"] = wg.reshape(2, 128, 10 * 1024)
    out["wa2"] = np.concatenate([np.asarray(inp["gla_w_a2"], f), np.asarray(inp["gla_b_a"], f)[:, None, :]], axis=1)
    wu = np.asarray(inp["ffn_w_up"], f).reshape(4, 8, 128, 2, 22, 128)
    out["wup"] = np.ascontiguousarray(wu.transpose(0, 4, 2, 1, 3, 5).reshape(4, 22, 128, 8 * 256))
    wd = np.asarray(inp["ffn_w_down"], f).reshape(4, 22, 128, 8, 128)
    out["wdn"] = np.ascontiguousarray(wd.transpose(0, 3, 2, 1, 4).reshape(4, 8, 128, 22 * 128))
    return out


_CACHE = {}


def _get_nc(layers):
    key = tuple(layers)
    if key not in _CACHE:
        _CACHE[key] = Builder(layers).build()
    return _CACHE[key]


def _run(layers, xT_list, memT_list, w):
    nc = _get_nc(layers)
    in_maps = []
    for c in range(8):
        m = dict(w)
        m["xT"] = xT_list[c]
        m["memT"] = memT_list[c]
        in_maps.append(m)
    res = run_bass_kernel_spmd(nc, in_maps, core_ids=list(range(8)))
    return [np.asarray(r["yT"]) for r in res.results]


LAUNCH_GROUPS = [[0, 1, 2, 3]]


def kernel(**inputs):
    x = np.asarray(inputs["x"], np.float32)
    mem = np.asarray(inputs["mem"], np.float32)
    w = _prep_weights(inputs)
    xT = [np.ascontiguousarray(x[b].T) for b in range(8)]
    memT = [np.ascontiguousarray(mem[b].T) for b in range(8)]
    for grp in LAUNCH_GROUPS:
        xT = _run(grp, xT, memT, w)
    return np.stack([np.ascontiguousarray(y.T) for y in xT], axis=0).astype(np.float32)
```
